# Optimizing a Trainium2 kernel written in Bass

```python
import math
import jax, jax.numpy as jnp
from jax import lax
import numpy as np

D_MODEL = 1024
BATCH = 8
SEQ = 2048
DEPTH = 1

ATT_HEADS = 8
ATT_HEAD_DIM = 64
KV_RANK = 128
IDX_HEADS = 8
IDX_DIM = 32
TOPK_MAX = 256
Q_BLOCK = 128
N_BUCKETS = 32
MAX_DISTANCE = 128
DN_HEADS = 4
DN_HEAD_DIM = 128
CONV_WIDTH = 4
CHUNK = 64
ATT_W = ATT_HEADS * ATT_HEAD_DIM
DN_W = DN_HEADS * DN_HEAD_DIM
MIX_WIDTH = ATT_W + DN_W
IN_SIZES = (ATT_W, KV_RANK, IDX_HEADS * IDX_DIM, IDX_DIM, IDX_HEADS, 3 * DN_W, DN_W, DN_HEADS, DN_HEADS)
IN_WIDTH = sum(IN_SIZES)
N_GROUPS = 4
EXPERTS_PER_GROUP = 8
N_EXPERTS = N_GROUPS * EXPERTS_PER_GROUP
TOP_K_IN_GROUP = 2
EXPERT_FF = D_MODEL // 2
EPS = 1e-6

kernel_name = "hybrid_dsa_gdn_hmoe_adaln"


def rms_norm(x, g):
    xf = x.astype(jnp.float32)
    y = xf * lax.rsqrt(jnp.mean(xf * xf, axis=-1, keepdims=True) + EPS)
    return (y * g.astype(jnp.float32)).astype(x.dtype)


def l2_norm(x):
    return x * lax.rsqrt(jnp.sum(x * x, axis=-1, keepdims=True) + EPS)


def t5_bucket(dist):
    n = jnp.maximum(dist, 0)
    max_exact = N_BUCKETS // 2
    nf = jnp.maximum(n, 1).astype(jnp.float32)
    large = max_exact + (jnp.log(nf / max_exact) / math.log(MAX_DISTANCE / max_exact)
                         * (N_BUCKETS - max_exact)).astype(jnp.int32)
    large = jnp.minimum(large, N_BUCKETS - 1)
    return jnp.where(n < max_exact, n, large)


def dsa_attention(q, k, v, q_idx, k_idx, w_idx, positions, rel_bias):
    B, T = q.shape[0], q.shape[1]
    n_sel = min(TOPK_MAX, T // 4)
    nb = T // Q_BLOCK

    def to_blocks(a):
        return jnp.moveaxis(a.reshape((B, nb, Q_BLOCK) + a.shape[2:]), 1, 0)

    t_ids = jnp.arange(T, dtype=jnp.int32).reshape(nb, Q_BLOCK)
    key_ids = jnp.arange(T, dtype=jnp.int32)
    scale = ATT_HEAD_DIM ** -0.5
    idx_scale = IDX_DIM ** -0.5
    k_idx_f = k_idx.astype(jnp.float32)
    gather = jax.vmap(lambda a, i: a[i])

    def block(args):
        qb, qib, wb, pb, tb = args
        s_idx = jnp.einsum('bqhd,bsd->bqhs', qib.astype(jnp.float32), k_idx_f) * idx_scale
        score = jnp.einsum('bqh,bqhs->bqs', wb.astype(jnp.float32), jax.nn.relu(s_idx))
        causal = key_ids[None, :] <= tb[:, None]
        score = jnp.where(causal[None], score, -jnp.inf)
        _, sel = lax.top_k(score, n_sel)
        valid = sel <= tb[None, :, None]
        k_sel = gather(k, sel)
        v_sel = gather(v, sel)
        p_sel = gather(positions, sel)
        bias = rel_bias[t5_bucket(pb[:, :, None] - p_sel)]
        logits = (jnp.einsum('bqhd,bqkd->bqhk', qb, k_sel).astype(jnp.float32) * scale
                  + jnp.moveaxis(bias.astype(jnp.float32), -1, 2))
        logits = jnp.where(valid[:, :, None, :], logits, -jnp.inf)
        probs = jax.nn.softmax(logits, axis=-1).astype(v.dtype)
        return jnp.einsum('bqhk,bqkd->bqhd', probs, v_sel)

    out = lax.map(block, (to_blocks(q), to_blocks(q_idx), to_blocks(w_idx), to_blocks(positions), t_ids))
    return jnp.moveaxis(out, 0, 1).reshape(B, T, ATT_W)


def gated_deltanet(qkv, z, beta_in, a_in, conv_w, a_log, dt_bias, norm_g):
    B, T = qkv.shape[0], qkv.shape[1]
    qkv = lax.conv_general_dilated(qkv, conv_w[:, None, :], window_strides=(1,),
                                   padding=((CONV_WIDTH - 1, 0),),
                                   dimension_numbers=('NWC', 'WIO', 'NWC'),
                                   feature_group_count=qkv.shape[-1])
    qkv = jax.nn.silu(qkv.astype(jnp.float32))
    q, k, v = jnp.split(qkv, 3, axis=-1)
    heads = lambda a: a.reshape(B, T, DN_HEADS, DN_HEAD_DIM)
    q = l2_norm(heads(q)) * DN_HEAD_DIM ** -0.5
    k = l2_norm(heads(k))
    v = heads(v)
    beta = jax.nn.sigmoid(beta_in.astype(jnp.float32))
    g = -jnp.exp(a_log.astype(jnp.float32)) * jax.nn.softplus(a_in.astype(jnp.float32) + dt_bias.astype(jnp.float32))
    nc = T // CHUNK

    def chunks(a):
        a = jnp.moveaxis(a, 2, 1)
        return a.reshape((B, DN_HEADS, nc, CHUNK) + a.shape[3:])

    q, k, v, beta, g = [chunks(a) for a in (q, k, v, beta, g)]
    G = jnp.cumsum(g, axis=-1)
    diff = G[..., :, None] - G[..., None, :]
    incl = jnp.tril(jnp.ones((CHUNK, CHUNK), dtype=bool))
    strict = jnp.tril(jnp.ones((CHUNK, CHUNK), dtype=bool), -1)
    decay_incl = jnp.exp(jnp.where(incl, diff, -jnp.inf))
    decay_strict = jnp.where(strict, decay_incl, 0.0)
    kk = jnp.einsum('bhncd,bhnsd->bhncs', k, k)
    a_mat = beta[..., None] * kk * decay_strict + jnp.eye(CHUNK, dtype=jnp.float32)
    rhs = jnp.concatenate([beta[..., None] * v, (beta * jnp.exp(G))[..., None] * k], axis=-1)
    sol = lax.linalg.triangular_solve(a_mat, rhs, left_side=True, lower=True, unit_diagonal=True)
    u, w = sol[..., :DN_HEAD_DIM], sol[..., DN_HEAD_DIM:]
    p = jnp.einsum('bhncd,bhnsd->bhncs', q, k) * decay_incl
    qg = q * jnp.exp(G)[..., None]
    g_last = G[..., -1]
    kd = k * jnp.exp(g_last[..., None] - G)[..., None]
    gc = jnp.exp(g_last)

    def step(S, xs):
        u_c, w_c, p_c, qg_c, kd_c, gc_c = xs
        delta = u_c - jnp.einsum('bhcd,bhde->bhce', w_c, S)
        o = jnp.einsum('bhcd,bhde->bhce', qg_c, S) + jnp.einsum('bhcs,bhse->bhce', p_c, delta)
        S = gc_c[..., None, None] * S + jnp.einsum('bhcd,bhce->bhde', kd_c, delta)
        return S, o

    S0 = jnp.zeros((B, DN_HEADS, DN_HEAD_DIM, DN_HEAD_DIM), jnp.float32)
    xs = tuple(jnp.moveaxis(a, 2, 0) for a in (u, w, p, qg, kd, gc))
    _, o = lax.scan(step, S0, xs)
    o = jnp.moveaxis(o, 0, 2).reshape(B, DN_HEADS, T, DN_HEAD_DIM).transpose(0, 2, 1, 3)
    zf = z.astype(jnp.float32).reshape(B, T, DN_HEADS, DN_HEAD_DIM)
    o = rms_norm(o, norm_g) * jax.nn.silu(zf)
    return o.reshape(B, T, DN_W)


def hier_moe(h, rg_w, rg_b, re_w, re_b, w1, w3, w2):
    B, T, D = h.shape
    hf = h.reshape(-1, D)
    g_prob = jax.nn.softmax((hf @ rg_w + rg_b).astype(jnp.float32), axis=-1)
    g_top, g_sel = lax.top_k(g_prob, 1)
    e_logits = (hf @ re_w + re_b).astype(jnp.float32).reshape(-1, N_GROUPS, EXPERTS_PER_GROUP)
    e_in_group = jnp.einsum('ng,nge->ne', jax.nn.one_hot(g_sel[:, 0], N_GROUPS, dtype=jnp.float32), e_logits)
    e_prob = jax.nn.softmax(e_in_group, axis=-1)
    e_top, e_sel = lax.top_k(e_prob, TOP_K_IN_GROUP)
    e_top = e_top / jnp.sum(e_top, axis=-1, keepdims=True)
    weights = g_top * e_top
    expert_id = g_sel * EXPERTS_PER_GROUP + e_sel
    gates = jnp.sum(jax.nn.one_hot(expert_id, N_EXPERTS, dtype=jnp.float32) * weights[..., None], axis=1)
    gates = gates.astype(h.dtype)
    y = jnp.zeros_like(hf)
    for e in range(N_EXPERTS):
        act = jax.nn.silu(hf @ w1[e]) * (hf @ w3[e])
        y = y + gates[:, e:e + 1] * (act @ w2[e])
    return y.reshape(B, T, D)


def setup_inputs(seed: int = 0) -> dict:
    key = jax.random.key(seed)
    ks = jax.random.split(key, 32)
    f32 = jnp.float32
    L = DEPTH
    nrm = lambda k, shape, s: jax.random.normal(k, shape, f32) * s
    gain = lambda k, n: 1.0 + 0.02 * jax.random.normal(k, (L, n), f32)
    dt = jnp.exp(jax.random.uniform(ks[15], (L, DN_HEADS), f32, math.log(1e-3), math.log(1e-1)))
    return {
        "x": nrm(ks[0], (BATCH, SEQ, D_MODEL), 1.0),
        "c": nrm(ks[1], (BATCH, D_MODEL), 1.0),
        "positions": jnp.broadcast_to(jnp.arange(SEQ, dtype=jnp.int32), (BATCH, SEQ)),
        "ada_w": nrm(ks[2], (L, D_MODEL, 6 * D_MODEL), 0.5 * D_MODEL ** -0.5),
        "ada_b": nrm(ks[3], (L, 6 * D_MODEL), 0.02),
        "norm1_g": gain(ks[4], D_MODEL),
        "norm2_g": gain(ks[5], D_MODEL),
        "w_in": nrm(ks[6], (L, D_MODEL, IN_WIDTH), D_MODEL ** -0.5),
        "q_norm_g": gain(ks[7], ATT_HEAD_DIM),
        "k_norm_g": gain(ks[8], ATT_HEAD_DIM),
        "kv_norm_g": gain(ks[9], KV_RANK),
        "w_kv_up": nrm(ks[10], (L, KV_RANK, 2 * ATT_HEAD_DIM), KV_RANK ** -0.5),
        "idx_k_norm_g": gain(ks[11], IDX_DIM),
        "rel_bias": nrm(ks[12], (N_BUCKETS, ATT_HEADS), 0.5),
        "conv_w": nrm(ks[13], (L, CONV_WIDTH, 3 * DN_W), CONV_WIDTH ** -0.5),
        "a_log": jnp.log(jax.random.uniform(ks[14], (L, DN_HEADS), f32, 1.0, 16.0)),
        "dt_bias": dt + jnp.log(-jnp.expm1(-dt)),
        "dn_norm_g": gain(ks[16], DN_HEAD_DIM),
        "w_out": nrm(ks[17], (L, MIX_WIDTH, D_MODEL), MIX_WIDTH ** -0.5),
        "router_g_w": nrm(ks[18], (L, D_MODEL, N_GROUPS), D_MODEL ** -0.5),
        "router_g_b": nrm(ks[19], (L, N_GROUPS), 0.01),
        "router_e_w": nrm(ks[20], (L, D_MODEL, N_EXPERTS), D_MODEL ** -0.5),
        "router_e_b": nrm(ks[21], (L, N_EXPERTS), 0.01),
        "w1": nrm(ks[22], (L, N_EXPERTS, D_MODEL, EXPERT_FF), D_MODEL ** -0.5),
        "w3": nrm(ks[23], (L, N_EXPERTS, D_MODEL, EXPERT_FF), D_MODEL ** -0.5),
        "w2": nrm(ks[24], (L, N_EXPERTS, EXPERT_FF, D_MODEL), EXPERT_FF ** -0.5),
    }


def reference(x, c, positions, ada_w, ada_b, norm1_g, norm2_g, w_in, q_norm_g, k_norm_g,
              kv_norm_g, w_kv_up, idx_k_norm_g, rel_bias, conv_w, a_log, dt_bias, dn_norm_g,
              w_out, router_g_w, router_g_b, router_e_w, router_e_b, w1, w3, w2):
    B, T, D = x.shape
    split_pts = np.cumsum(IN_SIZES)[:-1].tolist()
    for l in range(DEPTH):
        mod = jax.nn.silu(c) @ ada_w[l] + ada_b[l]
        shift1, scale1, gate1, shift2, scale2, gate2 = [m[:, None, :] for m in jnp.split(mod, 6, axis=-1)]

        h = rms_norm(x, norm1_g[l]) * (1.0 + scale1) + shift1
        proj = h @ w_in[l]
        q_att, kv_lat, q_idx, k_idx, w_idx, dn_qkv, dn_z, dn_beta, dn_a = jnp.split(proj, split_pts, axis=-1)
        q = rms_norm(q_att.reshape(B, T, ATT_HEADS, ATT_HEAD_DIM), q_norm_g[l])
        kv = rms_norm(kv_lat, kv_norm_g[l]) @ w_kv_up[l]
        k = rms_norm(kv[..., :ATT_HEAD_DIM], k_norm_g[l])
        v = kv[..., ATT_HEAD_DIM:]
        qi = q_idx.reshape(B, T, IDX_HEADS, IDX_DIM)
        ki = rms_norm(k_idx, idx_k_norm_g[l])
        wi = w_idx * IDX_HEADS ** -0.5
        att = dsa_attention(q, k, v, qi, ki, wi, positions, rel_bias)
        dn = gated_deltanet(dn_qkv, dn_z, dn_beta, dn_a, conv_w[l], a_log[l], dt_bias[l], dn_norm_g[l])
        mixed = jnp.concatenate([att.astype(x.dtype), dn.astype(x.dtype)], axis=-1) @ w_out[l]
        x = x + gate1 * mixed

        h2 = rms_norm(x, norm2_g[l]) * (1.0 + scale2) + shift2
        x = x + gate2 * hier_moe(h2, router_g_w[l], router_g_b[l], router_e_w[l], router_e_b[l],
                                 w1[l], w3[l], w2[l])
    return x
```

```python
from contextlib import ExitStack
import math
import numpy as np
import concourse.bass as bass
import concourse.mybir as mybir
from concourse.bass_utils import run_bass_kernel_spmd

F32 = mybir.dt.float32
BF16 = mybir.dt.bfloat16
AF = mybir.ActivationFunctionType
ALU = mybir.AluOpType
AX = mybir.AxisListType

T = 2048
D = 1024
NT = 16
EPS = 1e-6
INW = 2992
C_QATT, C_KV, C_QIDX, C_KIDX, C_WIDX, C_DNQKV, C_DNZ, C_BETA, C_A = 0, 512, 640, 896, 928, 936, 2472, 2984, 2988
K_ITERS = 18
NEG = -1.0e30


class Buf:
    __slots__ = ("name", "t", "lw", "rd", "excl")

    def __init__(self, name, t):
        self.name = name
        self.t = t
        self.lw = None
        self.rd = []
        self.excl = False

    def __getitem__(self, idx):
        return self.t[idx]


class Eng:
    def __init__(self, name, h, sem):
        self.name, self.h, self.sem, self.cnt, self.seen = name, h, sem, 0, {}


class Ctx:
    def __init__(self, nc, n_dma_sems=32):
        self.nc = nc
        self.es = ExitStack()
        self.stacks = [self.es]
        self.E = {}
        for nm, h in (("pe", nc.tensor), ("act", nc.scalar), ("dve", nc.vector),
                      ("pool", nc.gpsimd), ("sp", nc.sync)):
            sem = self.es.enter_context(nc.semaphore("s_" + nm))
            self.E[nm] = Eng(nm, h, sem)
        self.dma_sems = []
        self.dma_pools = {"hw": [], "sw": []}
        for i in range(n_dma_sems):
            sem = self.es.enter_context(nc.semaphore("s_dma%d" % i))
            slot = [sem, 0]
            self.dma_sems.append(slot)
            self.dma_pools["hw" if i % 2 == 0 else "sw"].append(slot)
        self.dma_rr = {"hw": 0, "sw": 0}
        self.uid = 0

    def push(self):
        st = ExitStack()
        self.stacks.append(st)
        return st

    def pop(self):
        self.barrier()
        self.stacks.pop().close()

    def sb(self, name, shape, dt=F32):
        self.uid += 1
        t = self.stacks[-1].enter_context(self.nc.sbuf_tensor("%s_%d" % (name, self.uid), list(shape), dt))
        return Buf(name, t)

    def ps(self, name, shape, dt=F32):
        t = self.stacks[-1].enter_context(self.nc.psum_tensor(name, list(shape), dt))
        return Buf(name, t)

    def view(self, buf, name=None):
        return Buf(name or buf.name + "_v", buf.t)

    def _wait(self, eng, tok):
        if tok is None:
            return
        sem, val = tok
        key = id(sem)
        if eng.seen.get(key, 0) >= val:
            return
        eng.h.wait_ge(sem, val)
        eng.seen[key] = val

    def _deps(self, eng, reads, writes):
        own = id(eng.sem)
        for b in reads:
            self._wait(eng, b.lw)
            if b.excl:
                for tok in b.rd:
                    if id(tok[0]) != own:
                        self._wait(eng, tok)
        for b in writes:
            if b.lw is not None and id(b.lw[0]) != own:
                self._wait(eng, b.lw)
            for tok in b.rd:
                if id(tok[0]) != own:
                    self._wait(eng, tok)

    def _commit(self, tok, reads, writes):
        for b in reads:
            b.rd.append(tok)
            if len(b.rd) > 48:
                d = {}
                for s, v in b.rd:
                    k = id(s)
                    if k not in d or d[k][1] < v:
                        d[k] = (s, v)
                b.rd = list(d.values())
        for b in writes:
            b.lw = tok
            b.rd = []

    def op(self, en, fn, reads=(), writes=()):
        eng = self.E[en]
        self._deps(eng, reads, writes)
        ins = fn(eng.h)
        eng.cnt += 1
        ins.then_inc(eng.sem, 1)
        tok = (eng.sem, eng.cnt)
        self._commit(tok, reads, writes)
        return tok

    def dma(self, en, out, in_, reads=(), writes=(), **kw):
        eng = self.E[en]
        self._deps(eng, reads, writes)
        kind = "sw" if en == "pool" else "hw"
        pool_ = self.dma_pools[kind]
        slot = pool_[self.dma_rr[kind]]
        self.dma_rr[kind] = (self.dma_rr[kind] + 1) % len(pool_)
        sem, val = slot
        if val:
            self._wait(eng, (sem, val))
        ins = eng.h.dma_start(out=out, in_=in_, **kw)
        slot[1] = val + 16
        ins.then_inc(sem, 16)
        tok = (sem, val + 16)
        self._commit(tok, reads, writes)
        return tok

    def barrier(self):
        for e in self.E.values():
            for f in self.E.values():
                if f is not e and f.cnt:
                    self._wait(e, (f.sem, f.cnt))
            for sem, val in self.dma_sems:
                if val:
                    self._wait(e, (sem, val))

    def close(self):
        while self.stacks:
            self.stacks.pop().close()


def bc(ap, shape):
    return ap.to_broadcast(list(shape))


def t5_bucket_np(n):
    n = np.maximum(n, 0).astype(np.int32)
    nf = np.maximum(n, 1).astype(np.float32)
    large = 16 + (np.log(nf / np.float32(16)) / np.float32(math.log(128 / 16)) * np.float32(16)).astype(np.int32)
    large = np.minimum(large, 31)
    return np.where(n < 16, n, large)


def bucket_onehot():
    oh = np.zeros((32, 384), np.float32)
    n = np.arange(384) - 127
    b = t5_bucket_np(n)
    for m_ in range(384):
        if n[m_] >= 0:
            oh[b[m_], m_] = 1.0
    return oh


def bucket_lohi():
    b = t5_bucket_np(np.arange(0, 4096))
    lo = np.zeros(32, np.float32)
    hi = np.zeros(32, np.float32)
    for k in range(32):
        idx = np.nonzero(b == k)[0]
        lo[k] = idx.min()
        hi[k] = idx.max() + 1
    hi[31] = 1.0e9
    return np.concatenate([lo, hi]).astype(np.float32)


def build(stop_after=None, dbg=()):
    nc = bass.Bass("TRN2", target_bir_lowering=False)

    def din(name, shape, dt=F32):
        return nc.dram_tensor(name, list(shape), dt, kind="ExternalInput").ap()

    x_d = din("x", [T, D])
    c_d = din("c_fm", [128, 8])
    ada_w = din("ada_w", [D, 6 * D])
    ada_b = din("ada_b", [6 * D])
    n1g = din("norm1_g", [D])
    n2g = din("norm2_g", [D])
    w_in = din("w_in", [D, INW])
    qng = din("q_norm_g", [64])
    kng = din("k_norm_g", [64])
    kvng = din("kv_norm_g", [128])
    wkv = din("w_kv_up", [128, 128])
    ikng = din("idx_k_norm_g", [32])
    relb = din("rel_bias", [256])
    cw_d = din("conv_w_fm", [128, 12, 4])
    alog = din("a_log", [4])
    dtb = din("dt_bias", [4])
    dng = din("dn_norm_g", [128])
    w_out = din("w_out", [D, D])
    rw_d = din("router_w", [D, 36])
    rb_d = din("router_b", [36])
    w1 = din("w1", [32, D, 512])
    w3 = din("w3", [32, D, 512])
    w2 = din("w2", [32, 512, D])
    ohn_d = din("bk_ohn", [32, 384])
    out_d = nc.dram_tensor("out", [T, D], F32, kind="ExternalOutput").ap()
    dbg_d = {}

    c = Ctx(nc)
    op, dma = c.op, c.dma
    w_in_v = w_in.rearrange("(j p) n -> p j n", p=128)

    PT = [c.ps("ps%d" % i, [128, 1024]) for i in range(4)]
    PB = []
    for i in range(8):
        PB.append(c.view(PT[i // 2], "bank%d" % i))
        PB[-1].excl = True

    def bank(i, rows=128, c0=0, c1=512):
        return PT[i // 2].t[0:rows, (i % 2) * 512 + c0:(i % 2) * 512 + c1]

    def bank_bf(i, rows=128, n=1024):
        return PT[i // 2].t[0:rows, (i % 2) * 512:(i % 2) * 512 + 512].bitcast(BF16)[:, 0:n]

    identF = c.sb("identF", [128, 128])
    identB = c.sb("identB", [128, 128], BF16)
    onesF = c.sb("onesF", [128, 128])
    epsT = c.sb("epsT", [128, 1])
    op("pool", lambda h: h.memset(onesF[:], 1.0), writes=[onesF])
    op("pool", lambda h: h.memset(epsT[:], EPS), writes=[epsT])
    op("pool", lambda h: h.memset(identF[:], 1.0), writes=[identF])
    op("pool", lambda h: h.affine_select(out=identF[:], in_=identF[:], pattern=[[-1, 128]], compare_op=ALU.is_equal,
                                         fill=0.0, base=0, channel_multiplier=1), reads=[identF], writes=[identF])
    op("dve", lambda h: h.tensor_copy(out=identB[:], in_=identF[:]), reads=[identF], writes=[identB])

    MODBC = c.sb("MODBC", [128, 6 * D])
    SH1, A1, GT1, SH2, A2, GT2 = [MODBC[:, k * D:(k + 1) * D] for k in range(6)]
    HT = c.sb("HT", [128, 8, T], BF16)
    HTv = [c.view(HT, "HT%d" % i) for i in range(NT)]

    def rstd_from_ss(ss_ap, out_ap, n, reads, writes_buf, tmp_buf, tmp_ap):
        op("act", lambda h: h.activation(out=tmp_ap, in_=ss_ap, func=AF.Sqrt, bias=epsT[0:tmp_ap.shape[0], :], scale=1.0 / n),
           reads=reads + [epsT], writes=[tmp_buf])
        op("dve", lambda h: h.reciprocal(out=out_ap, in_=tmp_ap), reads=[tmp_buf], writes=[writes_buf])

    c.push()
    cs = c.sb("cs", [128, 8])
    SCR = c.sb("SCR", [128, 8, 128])
    G1BC = c.sb("G1BC", [128, D])
    G2BC = c.sb("G2BC", [128, D])
    AW = [c.sb("AW%d" % k, [128, 8, 512]) for k in range(2)]
    dma("sp", cs[:], c_d, writes=[cs])
    dma("sp", MODBC[:], ada_b.partition_broadcast(128), writes=[MODBC])
    dma("sp", G1BC[:], n1g.partition_broadcast(128), writes=[G1BC])
    dma("sp", G2BC[:], n2g.partition_broadcast(128), writes=[G2BC])
    op("act", lambda h: h.activation(out=cs[:], in_=cs[:], func=AF.Silu), reads=[cs], writes=[cs])
    for j in range(8):
        op("dve", lambda h: h.tensor_scalar(out=SCR[:, j, :], in0=onesF[:], scalar1=cs[:, j:j + 1], scalar2=None, op0=ALU.mult),
           reads=[onesF, cs], writes=[SCR])
    ada_v = ada_w.rearrange("(j p) n -> p j n", p=128)
    for m in range(12):
        aw = AW[m % 2]
        dma("sp", aw[:], ada_v[:, :, m * 512:(m + 1) * 512], writes=[aw])
        b = m % 2
        for j in range(8):
            op("pe", lambda h: h.matmul(bank(b), lhsT=SCR[:, j, :], rhs=aw[:, j, :], start=(j == 0), stop=(j == 7)),
               reads=[SCR, aw], writes=[PB[b]])
        op("dve", lambda h: h.tensor_tensor(out=MODBC[:, m * 512:(m + 1) * 512], in0=bank(b), in1=MODBC[:, m * 512:(m + 1) * 512], op=ALU.add),
           reads=[PB[b], MODBC], writes=[MODBC])
    op("dve", lambda h: h.scalar_tensor_tensor(out=A1, in0=A1, scalar=1.0, in1=G1BC[:], op0=ALU.add, op1=ALU.mult),
       reads=[MODBC, G1BC], writes=[MODBC])
    op("dve", lambda h: h.scalar_tensor_tensor(out=A2, in0=A2, scalar=1.0, in1=G2BC[:], op0=ALU.add, op1=ALU.mult),
       reads=[MODBC, G2BC], writes=[MODBC])
    c.pop()

    def norm_front(xt_buf, xt_ap, i, A_ap, S_ap, ss_buf, tmpA, tmpB, hb, extra_reads=()):
        junk = tmpA
        op("act", lambda h: h.activation(out=junk[:], in_=xt_ap, func=AF.Square, accum_out=ss_buf[:, 0:1]),
           reads=[xt_buf] + list(extra_reads), writes=[junk, ss_buf])
        rstd_from_ss(ss_buf[:, 0:1], ss_buf[:, 2:3], D, [ss_buf], ss_buf, ss_buf, ss_buf[:, 1:2])
        op("dve", lambda h: h.scalar_tensor_tensor(out=tmpB[:], in0=xt_ap, scalar=ss_buf[:, 2:3], in1=A_ap, op0=ALU.mult, op1=ALU.mult),
           reads=[xt_buf, ss_buf, MODBC] + list(extra_reads), writes=[tmpB])
        op("pool", lambda h: h.tensor_tensor(out=hb[:], in0=tmpB[:], in1=S_ap, op=ALU.add), reads=[tmpB, MODBC], writes=[hb])

    def norm_back(i, hb, pbank):
        pb = bank_bf(pbank)
        for k in range(8):
            op("pe", lambda h: h.transpose(out=pb[:, k * 128:(k + 1) * 128], in_=hb[:, k * 128:(k + 1) * 128], identity=identB[:]),
               reads=[hb, identB], writes=[PB[pbank]])
        op("act", lambda h: h.activation(out=HT[:, :, i * 128:(i + 1) * 128], in_=pb.rearrange("p (k t) -> p k t", t=128), func=AF.Copy),
           reads=[PB[pbank]], writes=[HTv[i]])

    def norm_to_HT(xt_buf, xt_ap, i, A_ap, S_ap, ss_buf, tmpA, tmpB, hb, pbank, extra_reads=()):
        norm_front(xt_buf, xt_ap, i, A_ap, S_ap, ss_buf, tmpA, tmpB, hb, extra_reads)
        norm_back(i, hb, pbank)

    c.push()
    NR = 3
    XS = [c.sb("XS%d" % k, [128, D]) for k in range(NR)]
    SS = [c.sb("SS%d" % k, [128, 4]) for k in range(NR)]
    TA = c.sb("TA", [128, D])
    TB = [c.sb("TB%d" % k, [128, D]) for k in range(NR)]
    HB = [c.sb("HB%d" % k, [128, D], BF16) for k in range(NR)]

    def s2_front(i):
        xs = XS[i % NR]
        dma("sp", xs[:], x_d[i * 128:(i + 1) * 128, :], writes=[xs])
        norm_front(xs, xs[:], i, A1, SH1, SS[i % NR], TA, TB[i % NR], HB[i % NR])

    s2_front(0)
    for i in range(NT):
        if i + 1 < NT:
            s2_front(i + 1)
        norm_back(i, HB[i % NR], i % 2)
    c.pop()
    if "HT" in dbg:
        dbg_d["HT"] = (HT, nc.dram_tensor("dbg_HT", [128, 8 * T], BF16, kind="ExternalOutput").ap())

    c.push()
    DNT = c.sb("DNT", [128, 4, T], BF16)
    if "nogdn" in dbg:
        op("pool", lambda h: h.memset(DNT[:], 0.0), writes=[DNT])
    elif stop_after != "s2":
        gdn_stage(c, nc, locals())
    if "DNT" in dbg:
        dbg_d["DNT"] = (DNT, nc.dram_tensor("dbg_DNT", [128, 4 * T], BF16, kind="ExternalOutput").ap())
    if stop_after in ("s2", "gdn", "gdn1"):
        return finish(c, nc, dbg_d, out_d, None)

    attn_stage(c, nc, locals())
    if stop_after in ("attn", "attn1"):
        return finish(c, nc, dbg_d, out_d, None)
    c.pop()
    n_exp = 32 if "e2" not in dbg else 2
    moe_stage(c, nc, locals())
    return finish(c, nc, dbg_d, out_d, None)


def finish(c, nc, dbg_d, out_d, X):
    for name, (buf, dap) in dbg_d.items():
        shape = dap.shape
        c.dma("sp", dap, buf.t[:].rearrange("p a b -> p (a b)") if len(buf.t.shape) == 3 else buf.t[:], reads=[buf])
    c.barrier()
    c.close()
    return nc


def gdn_stage(c, nc, L):
    op, dma, bank, PB, PT = L["op"], L["dma"], L["bank"], L["PB"], L["PT"]
    identF, onesF, epsT, HT, HTv, DNT, w_in_v = L["identF"], L["onesF"], L["epsT"], L["HT"], L["HTv"], L["DNT"], L["w_in_v"]
    cw_d, alog, dtb, dng, dbg, dbg_d = L["cw_d"], L["alog"], L["dtb"], L["dng"], L["dbg"], L["dbg_d"]
    c.push()

    def pbs(q):
        return [PB[2 * q], PB[2 * q + 1]]

    def P3(q, rows, inner):
        return PT[q].t[0:rows, :].rearrange("p (n d) -> p n d", d=inner)

    TUi = c.sb("TUi", [64, 64]); TUs = c.sb("TUs", [64, 64]); TLs = c.sb("TLs", [64, 64]); selL = c.sb("selL", [64, 128])
    for t_, pat, cm, base, cmp_ in ((TUi, [[1, 64]], -1, 0, ALU.is_ge), (TUs, [[1, 64]], -1, -1, ALU.is_ge),
                                   (TLs, [[-1, 64]], 1, -1, ALU.is_ge), (selL, [[0, 128]], 1, -63, ALU.is_equal)):
        op("pool", lambda h: h.memset(t_[:], 1.0), writes=[t_])
        op("pool", lambda h: h.affine_select(out=t_[:], in_=t_[:], pattern=pat, compare_op=cmp_, fill=0.0, base=base,
                                             channel_multiplier=cm), reads=[t_], writes=[t_])
    I64 = identF[0:64, 0:64]
    CW = c.sb("CW", [128, 12, 4]); dma("sp", CW[:], cw_d, writes=[CW])
    DNG = c.sb("DNG", [64, 128]); dma("sp", DNG[:], dng.partition_broadcast(64), writes=[DNG])
    DTB = c.sb("DTB", [64, 4]); dma("sp", DTB[:], dtb.partition_broadcast(64), writes=[DTB])
    NA = c.sb("NA", [64, 4]); dma("sp", NA[:], alog.partition_broadcast(64), writes=[NA])
    op("act", lambda h: h.activation(out=NA[:], in_=NA[:], func=AF.Exp), reads=[NA], writes=[NA])
    op("dve", lambda h: h.tensor_scalar(out=NA[:], in0=NA[:], scalar1=-1.0, scalar2=None, op0=ALU.mult), reads=[NA], writes=[NA])

    WBA = c.sb("WBA", [128, 8, 8], BF16)
    dma("pool", WBA[:], w_in_v[:, :, C_BETA:C_BETA + 8], writes=[WBA])
    for n in range(32):
        for k in range(8):
            op("pe", lambda h: h.matmul(bank(0, 64, 8 * n, 8 * n + 8), lhsT=HT[:, k, 64 * n:64 * n + 64], rhs=WBA[:, k, :],
                                        start=(k == 0), stop=(k == 7)), reads=[HTv[n // 2], WBA], writes=[PB[0]])
    BAR = c.sb("BAR", [64, 32, 8]); BETA = c.sb("BETA", [64, 32, 4]); GG = c.sb("GG", [64, 32, 4])
    GC = c.sb("GC", [64, 32, 4]); EG = c.sb("EG", [64, 32, 4]); BEG = c.sb("BEG", [64, 32, 4])
    GCB = c.sb("GCB", [128, 32, 4]); EKD = c.sb("EKD", [64, 32, 4])
    op("act", lambda h: h.activation(out=BAR[:], in_=bank(0, 64, 0, 256).rearrange("p (n e) -> p n e", e=8), func=AF.Copy),
       reads=[PB[0]], writes=[BAR])
    op("act", lambda h: h.activation(out=BETA[:], in_=BAR[:, :, 0:4], func=AF.Sigmoid), reads=[BAR], writes=[BETA])
    op("dve", lambda h: h.tensor_tensor(out=GG[:], in0=BAR[:, :, 4:8], in1=bc(DTB[:].unsqueeze(1), [64, 32, 4]), op=ALU.add),
       reads=[BAR, DTB], writes=[GG])
    op("act", lambda h: h.activation(out=GG[:], in_=GG[:], func=AF.Exp), reads=[GG], writes=[GG])
    op("act", lambda h: h.activation(out=GG[:], in_=GG[:], func=AF.Ln, bias=1.0, scale=1.0), reads=[GG], writes=[GG])
    op("dve", lambda h: h.tensor_tensor(out=GG[:], in0=GG[:], in1=bc(NA[:].unsqueeze(1), [64, 32, 4]), op=ALU.mult),
       reads=[GG, NA], writes=[GG])
    GGf = GG[:].rearrange("p n h -> p (n h)")
    GCf = GC[:].rearrange("p n h -> p (n h)")
    op("pe", lambda h: h.matmul(bank(1, 64, 0, 128), lhsT=TUi[:], rhs=GGf, start=True, stop=True), reads=[TUi, GG], writes=[PB[1]])
    op("act", lambda h: h.activation(out=GCf, in_=bank(1, 64, 0, 128), func=AF.Copy), reads=[PB[1]], writes=[GC])
    op("act", lambda h: h.activation(out=EG[:], in_=GC[:], func=AF.Exp), reads=[GC], writes=[EG])
    op("dve", lambda h: h.tensor_tensor(out=BEG[:], in0=BETA[:], in1=EG[:], op=ALU.mult), reads=[BETA, EG], writes=[BEG])
    op("pe", lambda h: h.matmul(bank(2, 128, 0, 128), lhsT=selL[:], rhs=GCf, start=True, stop=True), reads=[selL, GC], writes=[PB[2]])
    op("act", lambda h: h.activation(out=GCB[:].rearrange("p n h -> p (n h)"), in_=bank(2, 128, 0, 128), func=AF.Exp),
       reads=[PB[2]], writes=[GCB])
    op("dve", lambda h: h.tensor_tensor(out=EKD[:].rearrange("p n h -> p (n h)"), in0=bank(2, 64, 0, 128), in1=GCf, op=ALU.subtract),
       reads=[PB[2], GC], writes=[EKD])
    op("act", lambda h: h.activation(out=EKD[:], in_=EKD[:], func=AF.Exp), reads=[EKD], writes=[EKD])

    NB = 8
    NTK = 64 * NB
    NQB = T // NTK

    def make_set(hs):
        B0 = 4 * hs
        PA, PBk = PT[2 * hs], PT[2 * hs + 1]
        pbA = [PB[B0], PB[B0 + 1]]
        pbB = [PB[B0 + 2], PB[B0 + 3]]
        sfx = "_%d" % hs
        WG = c.sb("WG" + sfx, [128, 8, 512], BF16)
        FRAW = c.sb("FRAW" + sfx, [128, NTK + 3])
        CV = [c.sb("CV%d%s" % (g, sfx), [128, NTK]) for g in range(3)]
        SZ = c.sb("SZ" + sfx, [128, NTK])
        BK = c.sb("BK" + sfx, [64, NB, 128]); KD = c.sb("KD" + sfx, [64, NB, 128]); BV = c.sb("BV" + sfx, [64, NB, 128])
        R = c.sb("R" + sfx, [64, NB, 64]); D1 = c.sb("D1" + sfx, [128, NTK]); E1 = c.sb("E1" + sfx, [128, NTK]); E2 = c.sb("E2" + sfx, [128, NTK])
        PTS = c.sb("PTS" + sfx, [64, NB, 64]); Tt = c.sb("Tt" + sfx, [64, NB, 64]); Mn = c.sb("Mn" + sfx, [64, NB, 64]); An = c.sb("An" + sfx, [64, NB, 64])
        U = c.sb("U" + sfx, [64, NB, 128]); WT = c.sb("WT" + sfx, [128, NTK]); O = c.sb("O" + sfx, [64, NB, 128]); OJ = c.sb("OJ" + sfx, [64, 128])
        Sst = [c.sb("Sst%d%s" % (k, sfx), [128, 128]) for k in range(2)]
        DL = [c.sb("DL%d%s" % (k, sfx), [64, 128]) for k in range(2)]
        MS = c.sb("MS" + sfx, [64, NB]); MS2 = c.sb("MS2" + sfx, [64, NB])
        v3 = lambda buf: buf[0:64, :].rearrange("p (n d) -> p n d", d=64)
        I64b = bc(I64.unsqueeze(1), [64, NB, 64])
        Rf = R[:].rearrange("p n s -> p (n s)")
        state = {"scur": 0}
        A3 = lambda inner: PA.t[0:64, 0:NB * inner].rearrange("p (n d) -> p n d", d=inner) if NB * inner <= 1024 else None
        bA0 = lambda rows, c0, c1: PA.t[0:rows, c0:c1]
        bA1 = lambda rows, c0, c1: PA.t[0:rows, 512 + c0:512 + c1]
        bB0 = lambda rows, c0, c1: PBk.t[0:rows, c0:c1]
        bB1 = lambda rows, c0, c1: PBk.t[0:rows, 512 + c0:512 + c1]
        w64 = lambda f: f(64, 0, 64 * NB).rearrange("p (n d) -> p n d", d=64)

        def batch(hd, qb):
            th = []
            t0 = NTK * qb
            n0 = NB * qb
            nsl = slice(n0, n0 + NB)
            tiles = [HTv[i] for i in range(max(0, (t0 - 3) // 128), (t0 + NTK) // 128)]
            QN, KN, VN = CV
            if qb == 0:
                def p_w():
                    cols = [C_DNQKV + hd * 128, C_DNQKV + 512 + hd * 128, C_DNQKV + 1024 + hd * 128, C_DNZ + hd * 128]
                    for gi, c0 in enumerate(cols):
                        dma("pool", WG[:, :, gi * 128:(gi + 1) * 128], w_in_v[:, :, c0:c0 + 128], writes=[WG])
                    state["scur"] = 0
                    op("pool", lambda h: h.memset(Sst[0][:], 0.0), writes=[Sst[0]])
                th.append(p_w)
            for g in range(3):
                def p_proj(g=g):
                    ch = g * 4 + hd
                    cv = CV[g]
                    if qb == 0:
                        op("pool", lambda h: h.memset(FRAW[:, 0:3], 0.0), writes=[FRAW])
                    else:
                        for k in range(8):
                            op("pe", lambda h: h.matmul(bA1(128, 0, 3), lhsT=WG[:, k, g * 128:(g + 1) * 128], rhs=HT[:, k, t0 - 3:t0],
                                                        start=(k == 0), stop=(k == 7)), reads=[WG] + tiles, writes=[pbA[1]])
                        op("act", lambda h: h.activation(out=FRAW[:, 0:3], in_=bA1(128, 0, 3), func=AF.Copy), reads=[pbA[1]], writes=[FRAW])
                    for k in range(8):
                        op("pe", lambda h: h.matmul(bA0(128, 0, NTK), lhsT=WG[:, k, g * 128:(g + 1) * 128], rhs=HT[:, k, t0:t0 + NTK], start=(k == 0), stop=(k == 7)),
                           reads=[WG] + tiles, writes=[pbA[0]])
                    op("act", lambda h: h.activation(out=FRAW[:, 3:3 + NTK], in_=bA0(128, 0, NTK), func=AF.Copy), reads=[pbA[0]], writes=[FRAW])
                    op("dve", lambda h: h.tensor_scalar(out=cv[:], in0=FRAW[:, 3:3 + NTK], scalar1=CW[:, ch, 3:4], scalar2=None, op0=ALU.mult), reads=[FRAW, CW], writes=[cv])
                    for j in (2, 1, 0):
                        op("dve", lambda h: h.scalar_tensor_tensor(out=cv[:], in0=FRAW[:, j:j + NTK], scalar=CW[:, ch, j:j + 1], in1=cv[:],
                                                                   op0=ALU.mult, op1=ALU.add), reads=[FRAW, CW, cv], writes=[cv])
                    op("act", lambda h: h.activation(out=cv[:], in_=cv[:], func=AF.Silu), reads=[cv], writes=[cv])
                th.append(p_proj)

            def p_z():
                for k in range(8):
                    op("pe", lambda h: h.matmul(bA0(128, 0, NTK), lhsT=WG[:, k, 384:512], rhs=HT[:, k, t0:t0 + NTK], start=(k == 0), stop=(k == 7)), reads=[WG] + tiles, writes=[pbA[0]])
                op("act", lambda h: h.activation(out=SZ[:], in_=bA0(128, 0, NTK), func=AF.Silu), reads=[pbA[0]], writes=[SZ])
            th.append(p_z)
            for g in range(2):
                def p_l2(g=g):
                    cv = CV[g]
                    op("pool", lambda h: h.tensor_tensor(out=D1[:], in0=cv[:], in1=cv[:], op=ALU.mult), reads=[cv], writes=[D1])
                    op("pe", lambda h: h.matmul(bA1(128, 0, NTK), lhsT=onesF[:], rhs=D1[:], start=True, stop=True), reads=[onesF, D1], writes=[pbA[1]])
                    op("act", lambda h: h.activation(out=E1[:], in_=bA1(128, 0, NTK), func=AF.Sqrt, bias=epsT[:], scale=1.0), reads=[pbA[1], epsT], writes=[E1])
                    op("dve", lambda h: h.reciprocal(out=E1[:], in_=E1[:]), reads=[E1], writes=[E1])
                    sc_ = 128.0 ** -0.5 if g == 0 else 1.0
                    op("dve", lambda h: h.scalar_tensor_tensor(out=cv[:], in0=E1[:], scalar=sc_, in1=cv[:], op0=ALU.mult, op1=ALU.mult), reads=[E1, cv], writes=[cv])
                th.append(p_l2)

            def p_kv():
                for n_ in range(NB):
                    op("pe", lambda h: h.transpose(out=PA.t[0:64, n_ * 128:n_ * 128 + 128], in_=KN[:, 64 * n_:64 * n_ + 64], identity=identF[:]),
                       reads=[KN, identF], writes=[pbA[n_ // 4]])
                    op("pe", lambda h: h.transpose(out=PBk.t[0:64, n_ * 128:n_ * 128 + 128], in_=VN[:, 64 * n_:64 * n_ + 64], identity=identF[:]),
                       reads=[VN, identF], writes=[pbB[n_ // 4]])
                pk = PA.t[0:64, :].rearrange("p (n d) -> p n d", d=128)
                pv = PBk.t[0:64, :].rearrange("p (n d) -> p n d", d=128)
                op("dve", lambda h: h.tensor_tensor(out=BK[:], in0=pk, in1=bc(BEG[:, nsl, hd:hd + 1], [64, NB, 128]), op=ALU.mult), reads=pbA + [BEG], writes=[BK])
                op("dve", lambda h: h.tensor_tensor(out=KD[:], in0=pk, in1=bc(EKD[:, nsl, hd:hd + 1], [64, NB, 128]), op=ALU.mult), reads=pbA + [EKD], writes=[KD])
                op("dve", lambda h: h.tensor_tensor(out=BV[:], in0=pv, in1=bc(BETA[:, nsl, hd:hd + 1], [64, NB, 128]), op=ALU.mult), reads=pbB + [BETA], writes=[BV])
            th.append(p_kv)
            d1 = v3(D1); e1 = v3(E1); e2 = v3(E2)

            def p_kk():
                for n_ in range(NB):
                    sl = slice(64 * n_, 64 * n_ + 64)
                    op("pe", lambda h: h.matmul(bA0(64, 64 * n_, 64 * n_ + 64), lhsT=KN[:, sl], rhs=KN[:, sl], start=True, stop=True), reads=[KN], writes=[pbA[0]])
                    op("pe", lambda h: h.matmul(bA1(64, 64 * n_, 64 * n_ + 64), lhsT=KN[:, sl], rhs=QN[:, sl], start=True, stop=True), reads=[KN, QN], writes=[pbA[1]])
                op("dve", lambda h: h.tensor_tensor(out=R[:], in0=bc(GC[:, nsl, hd:hd + 1], [64, NB, 64]), in1=I64b, op=ALU.mult), reads=[GC, identF], writes=[R])
                op("pe", lambda h: h.matmul(bB0(64, 0, NTK), lhsT=onesF[0:64, 0:64], rhs=Rf, start=True, stop=True), reads=[onesF, R], writes=[pbB[0]])
                op("dve", lambda h: h.tensor_tensor(out=d1, in0=w64(bB0), in1=bc(GC[:, nsl, hd:hd + 1], [64, NB, 64]), op=ALU.subtract), reads=[pbB[0], GC], writes=[D1])
                op("dve", lambda h: h.tensor_tensor(out=R[:], in0=bc(BETA[:, nsl, hd:hd + 1], [64, NB, 64]), in1=I64b, op=ALU.mult), reads=[BETA, identF], writes=[R])
                op("pe", lambda h: h.matmul(bB1(64, 0, NTK), lhsT=onesF[0:64, 0:64], rhs=Rf, start=True, stop=True), reads=[onesF, R], writes=[pbB[1]])
                op("dve", lambda h: h.tensor_scalar(out=e1, in0=d1, scalar1=0.0, scalar2=None, op0=ALU.min), reads=[D1], writes=[E1])
                op("dve", lambda h: h.tensor_scalar(out=e2, in0=d1, scalar1=-1.0, scalar2=0.0, op0=ALU.mult, op1=ALU.min), reads=[D1], writes=[E2])
                op("act", lambda h: h.activation(out=e1, in_=e1, func=AF.Exp), reads=[E1], writes=[E1])
                op("act", lambda h: h.activation(out=e2, in_=e2, func=AF.Exp), reads=[E2], writes=[E2])
            th.append(p_kk)

            def p_am():
                op("dve", lambda h: h.tensor_tensor(out=PTS[:], in0=w64(bA1), in1=e1, op=ALU.mult), reads=[pbA[1], E1], writes=[PTS])
                op("pool", lambda h: h.tensor_tensor(out=PTS[:], in0=PTS[:], in1=bc(TUi[:].unsqueeze(1), [64, NB, 64]), op=ALU.mult), reads=[PTS, TUi], writes=[PTS])
                op("dve", lambda h: h.tensor_tensor(out=e1, in0=w64(bA0), in1=e1, op=ALU.mult), reads=[pbA[0], E1], writes=[E1])
                op("pool", lambda h: h.tensor_tensor(out=e1, in0=e1, in1=bc(TUs[:].unsqueeze(1), [64, NB, 64]), op=ALU.mult), reads=[E1, TUs], writes=[E1])
                op("dve", lambda h: h.scalar_tensor_tensor(out=e1, in0=e1, scalar=-1.0, in1=w64(bB1), op0=ALU.mult, op1=ALU.mult), reads=[E1, pbB[1]], writes=[E1])
                op("dve", lambda h: h.tensor_tensor(out=e2, in0=w64(bA0), in1=e2, op=ALU.mult), reads=[pbA[0], E2], writes=[E2])
                op("pool", lambda h: h.tensor_tensor(out=e2, in0=e2, in1=bc(TLs[:].unsqueeze(1), [64, NB, 64]), op=ALU.mult), reads=[E2, TLs], writes=[E2])
                op("dve", lambda h: h.scalar_tensor_tensor(out=e2, in0=e2, scalar=-1.0, in1=bc(BETA[:, nsl, hd:hd + 1], [64, NB, 64]), op0=ALU.mult, op1=ALU.mult),
                   reads=[E2, BETA], writes=[E2])
                op("pool", lambda h: h.tensor_tensor(out=Tt[:], in0=e1, in1=I64b, op=ALU.add), reads=[E1, identF], writes=[Tt])
            th.append(p_am)
            cur = {"Ac": e1, "Mc": e2, "Acb": E1, "Mcb": E2, "Anx": An[:], "Mnx": Mn[:], "Anb": An, "Mnb": Mn}
            for lvl in range(1, 6):
                def p_lvl(lvl=lvl):
                    Ac, Mc, Acb, Mcb, Anx, Mnx, Anb, Mnb = (cur[k] for k in ("Ac", "Mc", "Acb", "Mcb", "Anx", "Mnx", "Anb", "Mnb"))
                    for n_ in range(NB):
                        op("pe", lambda h: h.matmul(bA0(64, 64 * n_, 64 * n_ + 64), lhsT=Ac[:, n_, :], rhs=Mc[:, n_, :], start=True, stop=True), reads=[Acb, Mcb], writes=[pbA[0]])
                    if lvl < 5:
                        for n_ in range(NB):
                            op("pe", lambda h: h.matmul(bA1(64, 64 * n_, 64 * n_ + 64), lhsT=Mc[:, n_, :], rhs=Ac[:, n_, :], start=True, stop=True), reads=[Acb, Mcb], writes=[pbA[1]])
                    op("act", lambda h: h.activation(out=Mnx, in_=w64(bA0), func=AF.Copy), reads=[pbA[0]], writes=[Mnb])
                    if lvl < 5:
                        op("dve", lambda h: h.tensor_copy(out=Anx, in_=w64(bA1)), reads=[pbA[1]], writes=[Anb])
                    for n_ in range(NB):
                        op("pe", lambda h: h.matmul(bB0(64, 64 * n_, 64 * n_ + 64), lhsT=Mnx[:, n_, :], rhs=Tt[:, n_, :], start=True, stop=True), reads=[Mnb, Tt], writes=[pbB[0]])
                    op("dve", lambda h: h.tensor_tensor(out=Tt[:], in0=Tt[:], in1=w64(bB0), op=ALU.add), reads=[Tt, pbB[0]], writes=[Tt])
                    cur.update(Ac=Anx, Mc=Mnx, Acb=Anb, Mcb=Mnb, Anx=Ac, Mnx=Mc, Anb=Acb, Mnb=Mcb)
                th.append(p_lvl)

            def p_sol():
                for n_ in range(NB):
                    op("pe", lambda h: h.matmul(PA.t[0:64, n_ * 128:n_ * 128 + 128], lhsT=Tt[:, n_, :], rhs=BV[:, n_, :], start=True, stop=True), reads=[Tt, BV], writes=[pbA[n_ // 4]])
                    op("pe", lambda h: h.matmul(bB0(128, 64 * n_, 64 * n_ + 64), lhsT=BK[:, n_, :], rhs=Tt[:, n_, :], start=True, stop=True), reads=[Tt, BK], writes=[pbB[0]])
                op("act", lambda h: h.activation(out=U[:], in_=PA.t[0:64, :].rearrange("p (n d) -> p n d", d=128), func=AF.Copy), reads=pbA, writes=[U])
                op("dve", lambda h: h.tensor_copy(out=WT[:], in_=bB0(128, 0, NTK)), reads=[pbB[0]], writes=[WT])
                op("dve", lambda h: h.tensor_tensor(out=R[:], in0=bc(EG[:, nsl, hd:hd + 1], [64, NB, 64]), in1=I64b, op=ALU.mult), reads=[EG, identF], writes=[R])
                op("pe", lambda h: h.matmul(bB1(128, 0, NTK), lhsT=onesF[0:64, :], rhs=Rf, start=True, stop=True), reads=[onesF, R], writes=[pbB[1]])
                op("dve", lambda h: h.tensor_tensor(out=QN[:], in0=QN[:], in1=bB1(128, 0, NTK), op=ALU.mult), reads=[QN, pbB[1]], writes=[QN])
            th.append(p_sol)
            for n_ in range(NB):
                def q_step(n_=n_):
                    n = n0 + n_
                    scur = state["scur"]
                    S0, S1 = Sst[scur], Sst[1 - scur]
                    dl = DL[n_ % 2]
                    sl = slice(64 * n_, 64 * n_ + 64)
                    op("pe", lambda h: h.matmul(bA0(64, 0, 128), lhsT=WT[:, sl], rhs=S0[:], start=True, stop=True), reads=[WT, S0], writes=[pbA[0]])
                    op("dve", lambda h: h.tensor_tensor(out=dl[:], in0=U[:, n_, :], in1=bA0(64, 0, 128), op=ALU.subtract), reads=[U, pbA[0]], writes=[dl])
                    op("pe", lambda h: h.matmul(bA1(64, 0, 128), lhsT=QN[:, sl], rhs=S0[:], start=True, stop=False), reads=[QN, S0], writes=[pbA[1]])
                    op("pe", lambda h: h.matmul(bA1(64, 0, 128), lhsT=PTS[:, n_, :], rhs=dl[:], start=False, stop=True), reads=[PTS, dl], writes=[pbA[1]])
                    op("act", lambda h: h.activation(out=O[:, n_, :], in_=bA1(64, 0, 128), func=AF.Copy), reads=[pbA[1]], writes=[O])
                    op("pe", lambda h: h.matmul(bB0(128, 0, 128), lhsT=KD[:, n_, :], rhs=dl[:], start=True, stop=True), reads=[KD, dl], writes=[pbB[0]])
                    op("dve", lambda h: h.scalar_tensor_tensor(out=S1[:], in0=S0[:], scalar=GCB[:, n, hd:hd + 1], in1=bB0(128, 0, 128), op0=ALU.mult, op1=ALU.add),
                       reads=[S0, GCB, pbB[0]], writes=[S1])
                    op("act", lambda h: h.activation(out=OJ[:], in_=O[:, n_, :], func=AF.Square, accum_out=MS[:, n_:n_ + 1]), reads=[O], writes=[OJ, MS])
                    state["scur"] = 1 - scur
                th.append(q_step)

            def q_out():
                op("act", lambda h: h.activation(out=MS2[:], in_=MS[:], func=AF.Sqrt, bias=epsT[0:64, :], scale=1.0 / 128), reads=[MS, epsT], writes=[MS2])
                op("dve", lambda h: h.reciprocal(out=MS2[:], in_=MS2[:]), reads=[MS2], writes=[MS2])
                op("dve", lambda h: h.tensor_tensor(out=O[:], in0=O[:], in1=bc(MS2[:].unsqueeze(2), [64, NB, 128]), op=ALU.mult), reads=[O, MS2], writes=[O])
                op("pool", lambda h: h.tensor_tensor(out=O[:], in0=O[:], in1=bc(DNG[:].unsqueeze(1), [64, NB, 128]), op=ALU.mult), reads=[O, DNG], writes=[O])
                for n_ in range(NB):
                    op("pe", lambda h: h.transpose(out=bB1(128, 64 * n_, 64 * n_ + 64), in_=O[:, n_, :], identity=I64), reads=[O, identF], writes=[pbB[1]])
                op("dve", lambda h: h.tensor_tensor(out=DNT[:, hd, t0:t0 + NTK], in0=bB1(128, 0, NTK), in1=SZ[:], op=ALU.mult), reads=[pbB[1], SZ], writes=[DNT])
            th.append(q_out)
            return th
        return batch

    batches = [make_set(0), make_set(1)]
    nqb = NQB if L.get("stop_after") != "gdn1" else 1
    la, lb = [], []
    for hh in range(2):
        for qb in range(nqb):
            la += batches[0](hh, qb)
            lb += batches[1](2 + hh, qb)
    OFF = L.get("gdn_off", 0)
    ia = ib = 0
    while ia < len(la) or ib < len(lb):
        if ia < len(la):
            la[ia]()
            ia += 1
        if ia > OFF or ia >= len(la):
            if ib < len(lb):
                lb[ib]()
                ib += 1
    c.pop()


def attn_stage(c, nc, L):
    op, dma, bank, bank_bf, PB, PT = L["op"], L["dma"], L["bank"], L["bank_bf"], L["PB"], L["PT"]
    identF, identB, onesF, epsT, HT, HTv, DNT, w_in_v = (L[k] for k in ("identF", "identB", "onesF", "epsT", "HT", "HTv", "DNT", "w_in_v"))
    MODBC, GT1, rstd_from_ss = L["MODBC"], L["GT1"], L["rstd_from_ss"]
    x_d, out_d, w_out, wkv, qng, kng, kvng, ikng, relb, ohn_d = (L[k] for k in ("x_d", "out_d", "w_out", "wkv", "qng", "kng", "kvng", "ikng", "relb", "ohn_d"))
    dbg, dbg_d, stop_after = L["dbg"], L["dbg_d"], L["stop_after"]
    c.push()

    def pbs(q):
        return [PB[2 * q], PB[2 * q + 1]]

    WTM2 = c.sb("WTM2", [128, 8, 160], BF16)
    dma("pool", WTM2[:, :, 0:128], w_in_v[:, :, C_KV:C_KV + 128], writes=[WTM2])
    dma("pool", WTM2[:, :, 128:160], w_in_v[:, :, C_KIDX:C_KIDX + 32], writes=[WTM2])
    WQ = c.sb("WQ", [128, 8, 520], BF16)
    dma("pool", WQ[:, :, 0:512], w_in_v[:, :, 0:512], writes=[WQ])
    dma("pool", WQ[:, :, 512:520], w_in_v[:, :, C_WIDX:C_WIDX + 8], writes=[WQ])
    WQI = c.sb("WQI", [128, 8, 256], BF16)
    dma("pool", WQI[:], w_in_v[:, :, C_QIDX:C_QIDX + 256], writes=[WQI])
    WKV = c.sb("WKV", [128, 128], BF16)
    dma("pool", WKV[:], wkv, writes=[WKV])
    WOA = c.sb("WOA", [64, 8, 1024], BF16)
    dma("pool", WOA[:], w_out[0:512, :].rearrange("(h d) n -> d h n", d=64), writes=[WOA])
    WOD = c.sb("WOD", [128, 4, 1024], BF16)
    dma("pool", WOD[:], w_out[512:1024, :].rearrange("(q p) n -> p q n", p=128), writes=[WOD])
    op("pool", lambda h: h.tensor_tensor(out=WOA[:], in0=WOA[:], in1=bc(MODBC[0:64, 2 * D:3 * D].unsqueeze(1), [64, 8, 1024]), op=ALU.mult), reads=[WOA, MODBC], writes=[WOA])
    op("pool", lambda h: h.tensor_tensor(out=WOD[:], in0=WOD[:], in1=bc(GT1.unsqueeze(1), [128, 4, 1024]), op=ALU.mult), reads=[WOD, MODBC], writes=[WOD])
    QG = c.sb("QG", [128, 64]); dma("sp", QG[:], qng.partition_broadcast(128), writes=[QG])
    KG = c.sb("KG", [128, 64]); dma("sp", KG[:], kng.partition_broadcast(128), writes=[KG])
    KVG = c.sb("KVG", [128, 128]); dma("sp", KVG[:], kvng.partition_broadcast(128), writes=[KVG])
    IKG = c.sb("IKG", [128, 32]); dma("sp", IKG[:], ikng.partition_broadcast(128), writes=[IKG])
    RBBC = c.sb("RBBC", [128, 256]); dma("sp", RBBC[:], relb.partition_broadcast(128), writes=[RBBC])
    CN = c.sb("CN", [128, 128])
    op("pool", lambda h: h.memset(CN[:], NEG), writes=[CN])
    op("pool", lambda h: h.affine_select(out=CN[:], in_=CN[:], pattern=[[1, 128]], compare_op=ALU.is_ge, fill=0.0, base=-1,
                                         channel_multiplier=-1), reads=[CN], writes=[CN])
    BTH = [c.sb("BTH%d" % k, [128, 1024], BF16) for k in range(2)]
    BTL = [c.sb("BTL%d" % k, [128, 1024], BF16) for k in range(2)]
    JB = c.sb("JB", [128, 128], BF16)
    op("pool", lambda h: h.memset(JB[:], 1.0), writes=[JB])
    op("pool", lambda h: h.affine_select(out=JB[:], in_=JB[:], pattern=[[1, 128]], compare_op=ALU.is_equal, fill=0.0, base=-127,
                                         channel_multiplier=1), reads=[JB], writes=[JB])
    c.push()
    RB32 = c.sb("RB32", [32, 8]); OHN = c.sb("OHN", [32, 384]); VT = c.sb("VT", [8, 384])
    BTF = c.sb("BTF", [128, 8, 128])
    dma("sp", RB32[:], relb.rearrange("(b h) -> b h", h=8), writes=[RB32])
    dma("sp", OHN[:], L["ohn_d"], writes=[OHN])
    op("dve", lambda h: h.tensor_scalar(out=RB32[:], in0=RB32[:], scalar1=8.0, scalar2=None, op0=ALU.mult), reads=[RB32], writes=[RB32])
    op("pe", lambda h: h.matmul(bank(0, 8, 0, 384), lhsT=RB32[:], rhs=OHN[:], start=True, stop=True), reads=[RB32, OHN], writes=[PB[0]])
    op("act", lambda h: h.activation(out=VT[:], in_=bank(0, 8, 0, 384), func=AF.Copy), reads=[PB[0]], writes=[VT])
    scr = nc.dram_tensor("t5_scr", [8, 384], F32)
    SCR = Buf("scr", scr)
    dma("sp", scr.ap(), VT[:], reads=[VT], writes=[SCR])
    for k in range(2):
        src = bass.AP(scr, 128 * k, [[1, 128], [384, 8], [1, 128]])
        dma("sp", BTF[:], src, reads=[SCR], writes=[BTF])
        btf = BTF[:].rearrange("p h t -> p (h t)")
        op("dve", lambda h: h.tensor_copy(out=BTH[k][:], in_=btf), reads=[BTF], writes=[BTH[k]])
        op("dve", lambda h: h.tensor_tensor(out=BTL[k][:], in0=btf, in1=BTH[k][:], op=ALU.subtract), reads=[BTF, BTH[k]], writes=[BTL[k]])
    c.pop()

    KT = c.sb("KT", [65, T], BF16)
    V1 = c.sb("V1", [128, NT, 128], BF16)
    KIT = c.sb("KIT", [96, T], BF16)
    op("pool", lambda h: h.memset(KT[64:65, :], 1.0), writes=[KT])
    op("pool", lambda h: h.memset(V1[:, :, 64:128], 1.0), writes=[V1])
    QT = [c.sb("QT%d" % k, [65, 8, 128], BF16) for k in range(3)]
    for k in range(3):
        op("dve", lambda h: h.tensor_scalar(out=QT[k][64:65, :, :], in0=bc(RBBC[64:65, 248:256].unsqueeze(2), [1, 8, 128]), scalar1=8.0, scalar2=None, op0=ALU.mult),
           reads=[RBBC], writes=[QT[k]])
    c.push()
    NSL = 4
    SSa_ = [c.sb("SSa%d" % q, [128, 16]) for q in range(NSL)]
    TM2_ = [c.sb("TM2%d" % q, [128, 160]) for q in range(NSL)]; JK_ = [c.sb("JK%d" % q, [128, 128]) for q in range(NSL)]
    KVL_ = [c.sb("KVL%d" % q, [128, 128], BF16) for q in range(NSL)]; KVLT_ = [c.sb("KVLT%d" % q, [128, 128], BF16) for q in range(NSL)]
    KVs_ = [c.sb("KVs%d" % q, [128, 128]) for q in range(NSL)]
    KB_ = [c.sb("KB%d" % q, [128, 64], BF16) for q in range(NSL)]; KIB_ = [c.sb("KIB%d" % q, [128, 96], BF16) for q in range(NSL)]

    def kv_tile(i, q):
        ts = slice(128 * i, 128 * i + 128)
        SSa, TM2, JK, KVL, KVLT, KVs, KB, KIB = SSa_[q], TM2_[q], JK_[q], KVL_[q], KVLT_[q], KVs_[q], KB_[q], KIB_[q]
        b0, b1 = 2 * q, 2 * q + 1
        th = []

        def k0():
            for k in range(8):
                op("pe", lambda h: h.matmul(bank(b0, 128, 0, 160), lhsT=HT[:, k, ts], rhs=WTM2[:, k, :], start=(k == 0), stop=(k == 7)), reads=[HTv[i], WTM2], writes=[PB[b0]])
            op("act", lambda h: h.activation(out=TM2[:], in_=bank(b0, 128, 0, 160), func=AF.Copy), reads=[PB[b0]], writes=[TM2])
        th.append(k0)

        def k1():
            op("act", lambda h: h.activation(out=JK[:, 0:128], in_=TM2[:, 0:128], func=AF.Square, accum_out=SSa[:, 0:1]), reads=[TM2], writes=[JK, SSa])
            op("act", lambda h: h.activation(out=JK[:, 0:32], in_=TM2[:, 128:160], func=AF.Square, accum_out=SSa[:, 8:9]), reads=[TM2], writes=[JK, SSa])
            rstd_from_ss(SSa[:, 0:1], SSa[:, 2:3], 128, [SSa], SSa, SSa, SSa[:, 1:2])
            rstd_from_ss(SSa[:, 8:9], SSa[:, 10:11], 32, [SSa], SSa, SSa, SSa[:, 9:10])
        th.append(k1)

        def k2():
            op("dve", lambda h: h.scalar_tensor_tensor(out=KVL[:], in0=TM2[:, 0:128], scalar=SSa[:, 2:3], in1=KVG[:], op0=ALU.mult, op1=ALU.mult), reads=[TM2, SSa, KVG], writes=[KVL])
            op("dve", lambda h: h.scalar_tensor_tensor(out=KIB[:].rearrange("p (r d) -> p r d", d=32), in0=bc(TM2[:, 128:160].unsqueeze(1), [128, 3, 32]), scalar=SSa[:, 10:11],
                                                       in1=bc(IKG[:].unsqueeze(1), [128, 3, 32]), op0=ALU.mult, op1=ALU.mult), reads=[TM2, SSa, IKG], writes=[KIB])
            op("pe", lambda h: h.transpose(out=bank_bf(b1, 128, 128), in_=KVL[:], identity=identB[:]), reads=[KVL, identB], writes=[PB[b1]])
            op("pe", lambda h: h.transpose(out=bank_bf(b1, 96, 256)[:, 128:256], in_=KIB[:], identity=identB[:]), reads=[KIB, identB], writes=[PB[b1]])
            op("act", lambda h: h.activation(out=KVLT[:], in_=bank_bf(b1, 128, 128), func=AF.Copy), reads=[PB[b1]], writes=[KVLT])
            op("act", lambda h: h.activation(out=KIT[:, ts], in_=bank_bf(b1, 96, 256)[:, 128:256], func=AF.Copy), reads=[PB[b1]], writes=[KIT])
        th.append(k2)

        def k3():
            op("pe", lambda h: h.matmul(bank(b0, 128, 256, 384), lhsT=KVLT[:], rhs=WKV[:], start=True, stop=True), reads=[KVLT, WKV], writes=[PB[b0]])
            op("act", lambda h: h.activation(out=KVs[:], in_=bank(b0, 128, 256, 384), func=AF.Copy), reads=[PB[b0]], writes=[KVs])
            op("pool", lambda h: h.tensor_copy(out=V1[:, i, 0:64], in_=KVs[:, 64:128]), reads=[KVs], writes=[V1])
            op("act", lambda h: h.activation(out=JK[:, 0:64], in_=KVs[:, 0:64], func=AF.Square, accum_out=SSa[:, 4:5]), reads=[KVs], writes=[JK, SSa])
            rstd_from_ss(SSa[:, 4:5], SSa[:, 6:7], 64, [SSa], SSa, SSa, SSa[:, 5:6])
        th.append(k3)

        def k4():
            op("dve", lambda h: h.scalar_tensor_tensor(out=KB[:], in0=KVs[:, 0:64], scalar=SSa[:, 6:7], in1=KG[:], op0=ALU.mult, op1=ALU.mult), reads=[KVs, SSa, KG], writes=[KB])
            op("pe", lambda h: h.transpose(out=bank_bf(b1, 64, 384)[:, 256:384], in_=KB[:], identity=identB[:]), reads=[KB, identB], writes=[PB[b1]])
            op("act", lambda h: h.activation(out=KT[0:64, ts], in_=bank_bf(b1, 64, 384)[:, 256:384], func=AF.Copy), reads=[PB[b1]], writes=[KT])
        th.append(k4)
        return th

    for i0 in range(0, NT, NSL):
        lists = [kv_tile(i0 + q, q) for q in range(NSL)]
        for step in range(len(lists[0])):
            for q in range(NSL):
                lists[q][step]()
    c.pop()
    if "KT" in dbg:
        dbg_d["KT"] = (KT, nc.dram_tensor("dbg_KT", [65, T], BF16, kind="ExternalOutput").ap())
        dbg_d["V1"] = (V1, nc.dram_tensor("dbg_V1", [128, NT * 128], BF16, kind="ExternalOutput").ap())
        dbg_d["KIT"] = (KIT, nc.dram_tensor("dbg_KIT", [96, T], BF16, kind="ExternalOutput").ap())

    TMQ = c.sb("TMQ", [128, 520]); QQ = c.sb("QQ", [128, 512]); QNB = c.sb("QNB", [128, 512], BF16)
    MSQ = c.sb("MSQ", [128, 24]); WV = c.sb("WV", [128, 8])
    QIT = c.sb("QIT", [96, 3, 128], BF16)
    S2 = [c.sb("S%d" % k, [128, T]) for k in range(2)]; MASK = c.sb("MASK", [128, T], BF16)
    MB = [c.sb("MB%d" % k, [128, NT, 128], BF16) for k in range(2)]
    RT = [c.sb("RT%d" % k, [128, 512]) for k in range(2)]
    ST = c.sb("ST", [128, 8]); WF = c.sb("WF", [128, K_ITERS]); FROW = c.sb("FROW", [128, K_ITERS])
    EB = [c.sb("EB%d" % k, [128, 1024], BF16) for k in range(2)]
    NDs = c.sb("NDs", [128, 1024]); DEN = c.sb("DEN", [64, 1024]); ATT = c.sb("ATT", [64, 8, 128], BF16)
    XT = c.sb("XT", [128, D]); TMP = c.sb("TMP", [128, D])
    WSC = (8.0 ** -0.5) * (32.0 ** -0.5)
    MBIG = 240000.0
    for k in range(K_ITERS):
        op("pool", lambda h: h.memset(FROW[:, k:k + 1], 2.0 * 2.0 ** -(k + 2)), writes=[FROW])
    last = NT if stop_after != "attn1" else 3

    def stageA(i, part):
        th = []
        ts = slice(128 * i, 128 * i + 128)
        Lk = 128 * (i + 1)
        qt = QT[i % 3]
        mb = MB[i % 2]
        S = S2[i % 2]

        def a0():
            for k in range(8):
                op("pe", lambda h: h.matmul(bank(6), lhsT=HT[:, k, ts], rhs=WQ[:, k, 0:512], start=(k == 0), stop=(k == 7)), reads=[HTv[i], WQ], writes=[PB[6]])
            for k in range(8):
                op("pe", lambda h: h.matmul(bank(7, 128, 0, 8), lhsT=HT[:, k, ts], rhs=WQ[:, k, 512:520], start=(k == 0), stop=(k == 7)), reads=[HTv[i], WQ], writes=[PB[7]])
            op("act", lambda h: h.activation(out=TMQ[:, 0:512], in_=bank(6), func=AF.Copy), reads=[PB[6]], writes=[TMQ])
            op("act", lambda h: h.activation(out=TMQ[:, 512:520], in_=bank(7, 128, 0, 8), func=AF.Copy), reads=[PB[7]], writes=[TMQ])
            op("pool", lambda h: h.tensor_tensor(out=QQ[:], in0=TMQ[:, 0:512], in1=TMQ[:, 0:512], op=ALU.mult), reads=[TMQ], writes=[QQ])
            op("dve", lambda h: h.tensor_reduce(out=MSQ[:, 0:8], in_=QQ[:].rearrange("p (h d) -> p h d", d=64), axis=AX.X, op=ALU.add), reads=[QQ], writes=[MSQ])
            rstd_from_ss(MSQ[:, 0:8], MSQ[:, 16:24], 64, [MSQ], MSQ, MSQ, MSQ[:, 8:16])
            op("dve", lambda h: h.tensor_tensor(out=QQ[:].rearrange("p (h d) -> p h d", d=64), in0=TMQ[:, 0:512].rearrange("p (h d) -> p h d", d=64),
                                                in1=bc(MSQ[:, 16:24].unsqueeze(2), [128, 8, 64]), op=ALU.mult), reads=[TMQ, MSQ], writes=[QQ])
            op("pool", lambda h: h.tensor_tensor(out=QNB[:].rearrange("p (h d) -> p h d", d=64), in0=QQ[:].rearrange("p (h d) -> p h d", d=64),
                                                 in1=bc(QG[:].unsqueeze(1), [128, 8, 64]), op=ALU.mult), reads=[QQ, QG], writes=[QNB])
            for hh in range(8):
                op("pe", lambda h: h.transpose(out=bank_bf(7, 64, 1024)[:, hh * 128:(hh + 1) * 128], in_=QNB[:, hh * 64:(hh + 1) * 64], identity=identB[:]),
                   reads=[QNB, identB], writes=[PB[7]])
            op("act", lambda h: h.activation(out=qt[0:64, :, :], in_=bank_bf(7, 64, 1024).rearrange("p (h t) -> p h t", t=128), func=AF.Copy), reads=[PB[7]], writes=[qt])
            op("dve", lambda h: h.tensor_scalar(out=WV[:], in0=TMQ[:, 512:520], scalar1=WSC, scalar2=None, op0=ALU.mult), reads=[TMQ], writes=[WV])
        if part == 1:
            th.append(a0)

        def a1():
            for grp in range(3):
                ncol = 96 if grp < 2 else 64
                for k in range(8):
                    op("pe", lambda h: h.matmul(bank(6, ncol, grp * 128, grp * 128 + 128), lhsT=WQI[:, k, grp * 96:grp * 96 + ncol], rhs=HT[:, k, ts],
                                                start=(k == 0), stop=(k == 7)), reads=[WQI, HTv[i]], writes=[PB[6]])
            op("act", lambda h: h.activation(out=QIT[0:96, 0:2, :], in_=bank(6, 96, 0, 256).rearrange("p (g t) -> p g t", t=128), func=AF.Copy), reads=[PB[6]], writes=[QIT])
            op("act", lambda h: h.activation(out=QIT[0:64, 2, :], in_=bank(6, 64, 256, 384), func=AF.Copy), reads=[PB[6]], writes=[QIT])
        if part == 1:
            th.append(a1)
        nch = (Lk + 511) // 512
        cnt_ = [0]
        for cc in range(nch):
            w = min(512, Lk - 512 * cc)
            for hh in range(8):
                def a2(cc=cc, w=w, hh=hh):
                    grp, r = hh // 3, hh % 3
                    bk = 6 + (cnt_[0] % 2)
                    rt = RT[cnt_[0] % 2]
                    cnt_[0] += 1
                    op("pe", lambda h: h.matmul(bank(bk, 128, 0, w), lhsT=QIT[32 * r:32 * r + 32, grp, :], rhs=KIT[32 * r:32 * r + 32, 512 * cc:512 * cc + w],
                                                start=True, stop=True), reads=[QIT, KIT], writes=[PB[bk]])
                    op("act", lambda h: h.activation(out=rt[:, 0:w], in_=bank(bk, 128, 0, w), func=AF.Relu), reads=[PB[bk]], writes=[rt])
                    if hh == 0:
                        op("dve", lambda h: h.tensor_scalar(out=S[:, 512 * cc:512 * cc + w], in0=rt[:, 0:w], scalar1=WV[:, 0:1], scalar2=None, op0=ALU.mult), reads=[rt, WV], writes=[S])
                    else:
                        op("dve", lambda h: h.scalar_tensor_tensor(out=S[:, 512 * cc:512 * cc + w], in0=rt[:, 0:w], scalar=WV[:, hh:hh + 1], in1=S[:, 512 * cc:512 * cc + w],
                                                                   op0=ALU.mult, op1=ALU.add), reads=[rt, WV, S], writes=[S])
                if part == 1:
                    th.append(a2)
        if part == 1:
            return th

        def a3():
            if i >= 2:
                op("dve", lambda h: h.tensor_reduce(out=ST[:, 0:1], in_=S[:, 0:Lk], axis=AX.X, op=ALU.max), reads=[S], writes=[ST])
                op("dve", lambda h: h.tensor_reduce(out=ST[:, 1:2], in_=S[:, 0:Lk], axis=AX.X, op=ALU.min), reads=[S], writes=[ST])
                op("dve", lambda h: h.tensor_scalar(out=ST[:, 2:3], in0=ST[:, 0:1], scalar1=ST[:, 1:2], scalar2=1.001, op0=ALU.subtract, op1=ALU.mult), reads=[ST], writes=[ST])
                op("dve", lambda h: h.tensor_tensor(out=WF[:], in0=FROW[:], in1=bc(ST[:, 2:3], [128, K_ITERS]), op=ALU.mult), reads=[FROW, ST], writes=[WF])
                op("dve", lambda h: h.scalar_tensor_tensor(out=ST[:, 4:5], in0=ST[:, 2:3], scalar=0.5, in1=ST[:, 1:2], op0=ALU.mult, op1=ALU.add), reads=[ST], writes=[ST])
            else:
                op("dve", lambda h: h.memset(ST[:, 3:4], -1.0e29), writes=[ST])
            op("dve", lambda h: h.tensor_tensor(out=S[:, ts], in0=S[:, ts], in1=CN[:], op=ALU.add), reads=[S, CN], writes=[S])
        th.append(a3)
        if i >= 2:
            for k in range(K_ITERS):
                def a4(k=k):
                    op("dve", lambda h: h.tensor_scalar(out=MASK[:, 0:Lk], in0=S[:, 0:Lk], scalar1=ST[:, 4:5], scalar2=0.0, op0=ALU.is_ge, op1=ALU.add, accum_out=ST[:, 5:6]),
                       reads=[S, ST], writes=[MASK, ST])
                    op("dve", lambda h: h.tensor_scalar(out=ST[:, 6:7], in0=ST[:, 5:6], scalar1=255.5, scalar2=0.5, op0=ALU.is_ge, op1=ALU.subtract), reads=[ST], writes=[ST])
                    if k < K_ITERS - 1:
                        op("dve", lambda h: h.scalar_tensor_tensor(out=ST[:, 4:5], in0=ST[:, 6:7], scalar=WF[:, k:k + 1], in1=ST[:, 4:5], op0=ALU.mult, op1=ALU.add), reads=[ST, WF], writes=[ST])
                    else:
                        op("dve", lambda h: h.tensor_scalar(out=ST[:, 6:7], in0=ST[:, 6:7], scalar1=0.5, scalar2=None, op0=ALU.subtract), reads=[ST], writes=[ST])
                        op("dve", lambda h: h.scalar_tensor_tensor(out=ST[:, 3:4], in0=ST[:, 6:7], scalar=WF[:, k:k + 1], in1=ST[:, 4:5], op0=ALU.mult, op1=ALU.add), reads=[ST, WF], writes=[ST])
                th.append(a4)

        def a5():
            op("dve", lambda h: h.tensor_scalar(out=MASK[:, 0:Lk], in0=S[:, 0:Lk], scalar1=ST[:, 3:4], scalar2=None, op0=ALU.is_ge), reads=[S, ST], writes=[MASK])
            if "S" in dbg and i == 2:
                dbg_d["S"] = (S, nc.dram_tensor("dbg_S", [128, T], F32, kind="ExternalOutput").ap())
                dbg_d["MASK"] = (MASK, nc.dram_tensor("dbg_MASK", [128, T], BF16, kind="ExternalOutput").ap())
            for j in range(i + 1):
                bk = 6 + j // 8
                op("pe", lambda h: h.transpose(out=bank_bf(bk)[:, (j % 8) * 128:(j % 8) * 128 + 128], in_=MASK[:, 128 * j:128 * j + 128], identity=identB[:]),
                   reads=[MASK, identB], writes=[PB[bk]])
            n6 = min(8, i + 1)
            op("act", lambda h: h.activation(out=mb[:, 0:n6, :], in_=bank_bf(6)[:, 0:n6 * 128].rearrange("p (j t) -> p j t", t=128), func=AF.Identity, scale=MBIG, bias=NEGB[:]),
               reads=[PB[6], NEGB], writes=[mb])
            if i >= 8:
                n7 = i + 1 - 8
                op("act", lambda h: h.activation(out=mb[:, 8:8 + n7, :], in_=bank_bf(7)[:, 0:n7 * 128].rearrange("p (j t) -> p j t", t=128), func=AF.Identity, scale=MBIG, bias=NEGB[:]),
                   reads=[PB[7], NEGB], writes=[mb])
        th.append(a5)
        return th

    def stageB(i):
        th = []
        ts = slice(128 * i, 128 * i + 128)
        qt = QT[i % 3]
        mb = MB[i % 2]
        qf = lambda K_, half: qt[0:K_, 4 * half:4 * half + 4, :].rearrange("p h t -> p (h t)")
        th.append(lambda: dma("sp", XT[:], x_d[ts, :], writes=[XT]))
        for j in range(i + 1):
            def b1(j=j):
                near = (i - j) <= 1
                K_ = 64 if near else 65
                pl = j % 2
                eb = EB[pl]
                for half in range(2):
                    o_ = PT[pl].t[:, 512 * half:512 * half + 512]
                    wr = [PB[2 * pl + half]]
                    op("pe", lambda h: h.matmul(o_, lhsT=KT[0:K_, 128 * j:128 * j + 128], rhs=qf(K_, half), start=True, stop=False), reads=[KT, qt], writes=wr)
                    op("pe", lambda h: h.matmul(o_, lhsT=identB[:], rhs=bc(mb[:, j, :].unsqueeze(1), [128, 4, 128]), start=False, stop=(not near)), reads=[identB, mb], writes=wr)
                    if near:
                        kk_ = i - j
                        op("pe", lambda h: h.matmul(o_, lhsT=JB[:], rhs=BTH[kk_][:, 512 * half:512 * half + 512], start=False, stop=False), reads=[JB, BTH[kk_]], writes=wr)
                        op("pe", lambda h: h.matmul(o_, lhsT=JB[:], rhs=BTL[kk_][:, 512 * half:512 * half + 512], start=False, stop=True), reads=[JB, BTL[kk_]], writes=wr)
                op("act", lambda h: h.activation(out=eb[:], in_=PT[pl].t[:, :], func=AF.Exp, scale=0.125), reads=pbs(pl), writes=[eb])
                for half in range(2):
                    op("pe", lambda h: h.matmul(PT[2].t[:, 512 * half:512 * half + 512], lhsT=V1[:, j, :], rhs=eb[:, 512 * half:512 * half + 512], start=(j == 0), stop=(j == i)),
                       reads=[V1, eb], writes=[PB[4 + half]])
            th.append(b1)

        def b2():
            op("act", lambda h: h.activation(out=NDs[0:64, :], in_=PT[2].t[0:64, :], func=AF.Copy), reads=pbs(2), writes=[NDs])
            op("act", lambda h: h.activation(out=NDs[64:128, :], in_=PT[2].t[64:128, :], func=AF.Ln), reads=pbs(2), writes=[NDs])
            op("act", lambda h: h.activation(out=NDs[64:128, :], in_=NDs[64:128, :], func=AF.Exp, scale=-1.0), reads=[NDs], writes=[NDs])
            dma("sp", DEN[:], NDs[64:128, :], reads=[NDs], writes=[DEN])
            op("pool", lambda h: h.tensor_tensor(out=ATT[:].rearrange("p h t -> p (h t)"), in0=NDs[0:64, :], in1=DEN[:], op=ALU.mult), reads=[NDs, DEN], writes=[ATT])
            if "ATT" in dbg and i == 2:
                dbg_d["ATT"] = (ATT, nc.dram_tensor("dbg_ATT", [64, 1024], BF16, kind="ExternalOutput").ap())
        th.append(b2)

        def b3():
            for half in range(2):
                for hh in range(8):
                    op("pe", lambda h: h.matmul(PT[0].t[:, 512 * half:512 * half + 512], lhsT=ATT[:, hh, :], rhs=WOA[:, hh, 512 * half:512 * half + 512], start=(hh == 0), stop=False),
                       reads=[ATT, WOA], writes=[PB[half]])
                for q_ in range(4):
                    op("pe", lambda h: h.matmul(PT[0].t[:, 512 * half:512 * half + 512], lhsT=DNT[:, q_, ts], rhs=WOD[:, q_, 512 * half:512 * half + 512], start=False, stop=(q_ == 3)),
                       reads=[DNT, WOD], writes=[PB[half]])
            op("act", lambda h: h.activation(out=TMP[:], in_=PT[0].t[:, :], func=AF.Copy), reads=pbs(0), writes=[TMP])
            op("pool", lambda h: h.tensor_tensor(out=XT[:], in0=TMP[:], in1=XT[:], op=ALU.add), reads=[TMP, XT], writes=[XT])
            dma("sp", out_d[ts, :], XT[:], reads=[XT])
        th.append(b3)
        return th

    NEGB = c.sb("NEGB", [128, 1])
    op("pool", lambda h: h.memset(NEGB[:], -MBIG), writes=[NEGB])
    def merge(lists):
        lists = [l for l in lists if l]
        if not lists:
            return
        n0 = len(lists[0])
        pos = [0] * len(lists)
        for ib in range(n0):
            lists[0][ib]()
            for q in range(1, len(lists)):
                tgt = ((ib + 1) * len(lists[q])) // n0
                while pos[q] < tgt:
                    lists[q][pos[q]]()
                    pos[q] += 1
        for q in range(1, len(lists)):
            while pos[q] < len(lists[q]):
                lists[q][pos[q]]()
                pos[q] += 1

    merge([stageA(0, 1)])
    merge([stageA(0, 2), stageA(1, 1) if last > 1 else []])
    for i in range(last):
        merge([stageB(i), stageA(i + 1, 2) if i + 1 < last else [], stageA(i + 2, 1) if i + 2 < last else []])
    c.pop()


def moe_stage(c, nc, L):
    op, dma, bank, bank_bf, PB, PT = L["op"], L["dma"], L["bank"], L["bank_bf"], L["PB"], L["PT"]
    HT, HTv, MODBC, A2, SH2, GT2, norm_to_HT = L["HT"], L["HTv"], L["MODBC"], L["A2"], L["SH2"], L["GT2"], L["norm_to_HT"]
    out_d, rw_d, rb_d, w1, w3, w2, dbg, dbg_d = (L[k] for k in ("out_d", "rw_d", "rb_d", "w1", "w3", "w2", "dbg", "dbg_d"))
    c.push()
    X = c.sb("X", [128, NT, D])
    Xv = [[c.view(X, "X%d_%d" % (i, dh)) for dh in range(2)] for i in range(NT)]
    GATES = c.sb("GATES", [128, NT, 4, 8])
    W1B = [c.sb("W1B%d" % k, [128, 8, 512], BF16) for k in range(2)]
    W3B = [c.sb("W3B%d" % k, [128, 8, 512], BF16) for k in range(2)]
    W2B = [c.sb("W2B%d" % k, [128, 4, 1024], BF16) for k in range(2)]
    n_exp = L.get("n_exp", 32)

    def load_expert(e):
        wb = e % 2
        dma("pool", W1B[wb][:], w1[e].rearrange("(j p) n -> p j n", p=128), writes=[W1B[wb]])
        dma("pool", W3B[wb][:], w3[e].rearrange("(j p) n -> p j n", p=128), writes=[W3B[wb]])
        dma("pool", W2B[wb][:], w2[e].rearrange("(j p) n -> p j n", p=128), writes=[W2B[wb]])
        op("pool", lambda h: h.tensor_tensor(out=W2B[wb][:], in0=W2B[wb][:], in1=bc(GT2.unsqueeze(1), [128, 4, 1024]), op=ALU.mult), reads=[W2B[wb], MODBC], writes=[W2B[wb]])

    for e in range(min(2, n_exp)):
        load_expert(e)
    c.push()
    RW = c.sb("RW", [128, 8, 36], BF16)
    dma("pool", RW[:], rw_d.rearrange("(j p) n -> p j n", p=128), writes=[RW])
    RBB = c.sb("RBB", [128, 36]); dma("sp", RBB[:], rb_d.partition_broadcast(128), writes=[RBB])
    LG = c.sb("LG", [128, NT, 36])
    NR = 3
    SS = [c.sb("SSm%d" % k, [128, 4]) for k in range(NR)]
    TA = c.sb("TAm", [128, D]); TB = [c.sb("TBm%d" % k, [128, D]) for k in range(NR)]
    HB = [c.sb("HBm%d" % k, [128, D], BF16) for k in range(NR)]
    norm_front, norm_back = L["norm_front"], L["norm_back"]

    def m_front(i):
        ts = slice(128 * i, 128 * i + 128)
        dma("sp", X[:, i, :], out_d[ts, :], writes=Xv[i])
        norm_front(Xv[i][0], X[:, i, :], i, A2, SH2, SS[i % NR], TA, TB[i % NR], HB[i % NR], extra_reads=[Xv[i][1]])

    m_front(0)
    for i in range(NT):
        ts = slice(128 * i, 128 * i + 128)
        if i + 1 < NT:
            m_front(i + 1)
        norm_back(i, HB[i % NR], i % 2)
        bk = 2 + i % 2
        for k in range(8):
            op("pe", lambda h: h.matmul(bank(bk, 128, 0, 36), lhsT=HT[:, k, ts], rhs=RW[:, k, :], start=(k == 0), stop=(k == 7)), reads=[HTv[i], RW], writes=[PB[bk]])
        op("dve", lambda h: h.tensor_tensor(out=LG[:, i, :], in0=bank(bk, 128, 0, 36), in1=RBB[:], op=ALU.add), reads=[PB[bk], RBB], writes=[LG])
    if "LG" in dbg:
        dbg_d["LG"] = (LG, nc.dram_tensor("dbg_LG", [128, NT * 36], F32, kind="ExternalOutput").ap())
    lg = LG[:, :, 0:4]
    le = LG[:, :, 4:36].rearrange("p i (g e) -> p i g e", e=8)
    GM = c.sb("GM", [128, NT]); GOH = c.sb("GOH", [128, NT, 4]); GE = c.sb("GE", [128, NT, 4]); GS = c.sb("GS", [128, NT])
    TG = c.sb("TG", [128, NT, 4, 8]); EIN = c.sb("EIN", [128, NT, 8]); EIN2 = c.sb("EIN2", [128, NT, 8])
    M1 = c.sb("M1", [128, NT]); M2 = c.sb("M2", [128, NT]); OH1 = c.sb("OH1r", [128, NT, 8]); OH2 = c.sb("OH2r", [128, NT, 8])
    E2 = c.sb("E2r", [128, NT]); W1c = c.sb("W1c", [128, NT]); W2c = c.sb("W2c", [128, NT]); GIG = c.sb("GIG", [128, NT, 8])
    dv = lambda fn, reads, writes: op("dve", fn, reads=reads, writes=writes)
    dv(lambda h: h.tensor_reduce(out=GM[:], in_=lg, axis=AX.X, op=ALU.max), [LG], [GM])
    dv(lambda h: h.tensor_tensor(out=GOH[:], in0=lg, in1=bc(GM[:].unsqueeze(2), [128, NT, 4]), op=ALU.is_ge), [LG, GM], [GOH])
    dv(lambda h: h.tensor_tensor(out=GE[:], in0=lg, in1=bc(GM[:].unsqueeze(2), [128, NT, 4]), op=ALU.subtract), [LG, GM], [GE])
    op("act", lambda h: h.activation(out=GE[:], in_=GE[:], func=AF.Exp), reads=[GE], writes=[GE])
    dv(lambda h: h.tensor_reduce(out=GS[:], in_=GE[:], axis=AX.X, op=ALU.add), [GE], [GS])
    dv(lambda h: h.reciprocal(out=GS[:], in_=GS[:]), [GS], [GS])
    dv(lambda h: h.tensor_tensor(out=TG[:], in0=le, in1=bc(GOH[:].unsqueeze(3), [128, NT, 4, 8]), op=ALU.mult), [LG, GOH], [TG])
    dv(lambda h: h.tensor_reduce(out=EIN[:], in_=TG[:].rearrange("p i g e -> p i e g"), axis=AX.X, op=ALU.add), [TG], [EIN])
    dv(lambda h: h.tensor_reduce(out=M1[:], in_=EIN[:], axis=AX.X, op=ALU.max), [EIN], [M1])
    dv(lambda h: h.tensor_tensor(out=OH1[:], in0=EIN[:], in1=bc(M1[:].unsqueeze(2), [128, NT, 8]), op=ALU.is_ge), [EIN, M1], [OH1])
    dv(lambda h: h.scalar_tensor_tensor(out=EIN2[:], in0=OH1[:], scalar=-1.0e30, in1=EIN[:], op0=ALU.mult, op1=ALU.add), [OH1, EIN], [EIN2])
    dv(lambda h: h.tensor_reduce(out=M2[:], in_=EIN2[:], axis=AX.X, op=ALU.max), [EIN2], [M2])
    dv(lambda h: h.tensor_tensor(out=OH2[:], in0=EIN2[:], in1=bc(M2[:].unsqueeze(2), [128, NT, 8]), op=ALU.is_ge), [EIN2, M2], [OH2])
    dv(lambda h: h.tensor_tensor(out=E2[:], in0=M2[:], in1=M1[:], op=ALU.subtract), [M1, M2], [E2])
    op("act", lambda h: h.activation(out=E2[:], in_=E2[:], func=AF.Exp), reads=[E2], writes=[E2])
    dv(lambda h: h.tensor_scalar(out=W1c[:], in0=E2[:], scalar1=1.0, scalar2=None, op0=ALU.add), [E2], [W1c])
    dv(lambda h: h.reciprocal(out=W1c[:], in_=W1c[:]), [W1c], [W1c])
    dv(lambda h: h.tensor_tensor(out=W1c[:], in0=W1c[:], in1=GS[:], op=ALU.mult), [W1c, GS], [W1c])
    dv(lambda h: h.tensor_tensor(out=W2c[:], in0=W1c[:], in1=E2[:], op=ALU.mult), [W1c, E2], [W2c])
    dv(lambda h: h.tensor_tensor(out=OH1[:], in0=OH1[:], in1=bc(W1c[:].unsqueeze(2), [128, NT, 8]), op=ALU.mult), [OH1, W1c], [OH1])
    dv(lambda h: h.tensor_tensor(out=OH2[:], in0=OH2[:], in1=bc(W2c[:].unsqueeze(2), [128, NT, 8]), op=ALU.mult), [OH2, W2c], [OH2])
    dv(lambda h: h.tensor_tensor(out=GIG[:], in0=OH1[:], in1=OH2[:], op=ALU.add), [OH1, OH2], [GIG])
    dv(lambda h: h.tensor_tensor(out=GATES[:], in0=bc(GOH[:].unsqueeze(3), [128, NT, 4, 8]), in1=bc(GIG[:].unsqueeze(2), [128, NT, 4, 8]), op=ALU.mult), [GOH, GIG], [GATES])
    if "GATES" in dbg:
        dbg_d["GATES"] = (GATES, nc.dram_tensor("dbg_GATES", [128, NT * 32], F32, kind="ExternalOutput").ap())
    c.pop()
    GAT = GATES[:].rearrange("p i g e -> p i (g e)")

    SL = [c.sb("SL%d" % k, [128, 512]) for k in range(2)]
    ACTT = [c.sb("ACTT%d" % k, [128, 4, 512], BF16) for k in range(2)]
    for e in range(n_exp):
        wb = e % 2
        if e >= 2:
            load_expert(e)
        for tb in range(4):
            at = ACTT[tb % 2]
            tiles = [HTv[4 * tb + q] for q in range(4)]
            for fc in range(4):
                bkA = (fc % 2) * 2
                bkB = bkA + 1
                sl_ = SL[fc % 2]
                for k in range(8):
                    op("pe", lambda h: h.matmul(bank(bkA), lhsT=W1B[wb][:, k, 128 * fc:128 * fc + 128], rhs=HT[:, k, 512 * tb:512 * tb + 512], start=(k == 0), stop=(k == 7)),
                       reads=[W1B[wb]] + tiles, writes=[PB[bkA]])
                for k in range(8):
                    op("pe", lambda h: h.matmul(bank(bkB), lhsT=W3B[wb][:, k, 128 * fc:128 * fc + 128], rhs=HT[:, k, 512 * tb:512 * tb + 512], start=(k == 0), stop=(k == 7)),
                       reads=[W3B[wb]] + tiles, writes=[PB[bkB]])
                op("act", lambda h: h.activation(out=sl_[:], in_=bank(bkA), func=AF.Silu), reads=[PB[bkA]], writes=[sl_])
                op("dve", lambda h: h.tensor_tensor(out=at[:, fc, :], in0=sl_[:], in1=bank(bkB), op=ALU.mult), reads=[sl_, PB[bkB]], writes=[at])
            for tt in range(4):
                tile = 4 * tb + tt
                for dh in range(2):
                    bkY = 4 + (2 * tt + dh) % 4
                    for fc in range(4):
                        op("pe", lambda h: h.matmul(bank(bkY), lhsT=at[:, fc, 128 * tt:128 * tt + 128], rhs=W2B[wb][:, fc, 512 * dh:512 * dh + 512], start=(fc == 0), stop=(fc == 3)),
                           reads=[at, W2B[wb]], writes=[PB[bkY]])
                    op("dve", lambda h: h.scalar_tensor_tensor(out=X[:, tile, 512 * dh:512 * dh + 512], in0=bank(bkY), scalar=GAT[:, tile, e:e + 1], in1=X[:, tile, 512 * dh:512 * dh + 512],
                                                               op0=ALU.mult, op1=ALU.add), reads=[PB[bkY], GATES, Xv[tile][dh]], writes=[Xv[tile][dh]])
    for i in range(NT):
        dma("sp", out_d[128 * i:128 * i + 128, :], X[:, i, :], reads=Xv[i])
    c.pop()


def make_inputs(inp, b):
    f = lambda a: np.ascontiguousarray(np.asarray(a, dtype=np.float32))
    m = {}
    m["x"] = f(inp["x"][b])
    m["c_fm"] = f(np.asarray(inp["c"][b]).reshape(8, 128).T)
    m["ada_w"] = f(inp["ada_w"][0])
    m["ada_b"] = f(inp["ada_b"][0])
    m["norm1_g"] = f(inp["norm1_g"][0])
    m["norm2_g"] = f(inp["norm2_g"][0])
    m["w_in"] = f(inp["w_in"][0])
    m["q_norm_g"] = f(inp["q_norm_g"][0])
    m["k_norm_g"] = f(inp["k_norm_g"][0])
    m["kv_norm_g"] = f(inp["kv_norm_g"][0])
    m["w_kv_up"] = f(inp["w_kv_up"][0])
    m["idx_k_norm_g"] = f(inp["idx_k_norm_g"][0])
    m["rel_bias"] = f(np.asarray(inp["rel_bias"]).reshape(256))
    m["conv_w_fm"] = f(np.asarray(inp["conv_w"][0]).reshape(4, 12, 128).transpose(2, 1, 0))
    m["a_log"] = f(inp["a_log"][0])
    m["dt_bias"] = f(inp["dt_bias"][0])
    m["dn_norm_g"] = f(inp["dn_norm_g"][0])
    m["w_out"] = f(inp["w_out"][0])
    m["router_w"] = f(np.concatenate([np.asarray(inp["router_g_w"][0]), np.asarray(inp["router_e_w"][0])], axis=1))
    m["router_b"] = f(np.concatenate([np.asarray(inp["router_g_b"][0]), np.asarray(inp["router_e_b"][0])], axis=0))
    m["w1"] = f(inp["w1"][0])
    m["w3"] = f(inp["w3"][0])
    m["w2"] = f(inp["w2"][0])
    m["bk_ohn"] = bucket_onehot()
    return m


def kernel(**inputs):
    nc = build()
    shared = None
    in_maps = []
    for b in range(8):
        m = make_inputs(inputs, b) if shared is None else dict(shared)
        if shared is None:
            shared = m
        else:
            m["x"] = np.ascontiguousarray(np.asarray(inputs["x"][b], dtype=np.float32))
            m["c_fm"] = np.ascontiguousarray(np.asarray(inputs["c"][b], dtype=np.float32).reshape(8, 128).T)
        in_maps.append(m)
    res = run_bass_kernel_spmd(nc, in_maps, core_ids=list(range(8)))
    return np.stack([np.asarray(r["out"], dtype=np.float32) for r in res.results], axis=0)
```

```python
from contextlib import ExitStack
import math
import numpy as np
import concourse.bass as bass
import concourse.mybir as mybir
from concourse.bass_utils import run_bass_kernel_spmd

F32 = mybir.dt.float32
BF16 = mybir.dt.bfloat16
AF = mybir.ActivationFunctionType
ALU = mybir.AluOpType
AX = mybir.AxisListType

T = 2048
D = 1024
NT = 16
EPS = 1e-6
INW = 2992
C_QATT, C_KV, C_QIDX, C_KIDX, C_WIDX, C_DNQKV, C_DNZ, C_BETA, C_A = 0, 512, 640, 896, 928, 936, 2472, 2984, 2988
K_ITERS = 18
NEG = -1.0e30


class Buf:
    __slots__ = ("name", "t", "lw", "rd", "excl")

    def __init__(self, name, t):
        self.name = name
        self.t = t
        self.lw = None
        self.rd = []
        self.excl = False

    def __getitem__(self, idx):
        return self.t[idx]


class Eng:
    def __init__(self, name, h, sem):
        self.name, self.h, self.sem, self.cnt, self.seen = name, h, sem, 0, {}


class Ctx:
    def __init__(self, nc, n_dma_sems=32):
        self.nc = nc
        self.es = ExitStack()
        self.stacks = [self.es]
        self.E = {}
        for nm, h in (("pe", nc.tensor), ("act", nc.scalar), ("dve", nc.vector),
                      ("pool", nc.gpsimd), ("sp", nc.sync)):
            sem = self.es.enter_context(nc.semaphore("s_" + nm))
            self.E[nm] = Eng(nm, h, sem)
        self.dma_sems = []
        self.dma_pools = {"hw": [], "sw": []}
        for i in range(n_dma_sems):
            sem = self.es.enter_context(nc.semaphore("s_dma%d" % i))
            slot = [sem, 0]
            self.dma_sems.append(slot)
            self.dma_pools["hw" if i % 2 == 0 else "sw"].append(slot)
        self.dma_rr = {"hw": 0, "sw": 0}
        self.uid = 0

    def push(self):
        st = ExitStack()
        self.stacks.append(st)
        return st

    def pop(self):
        self.barrier()
        self.stacks.pop().close()

    def sb(self, name, shape, dt=F32):
        self.uid += 1
        t = self.stacks[-1].enter_context(self.nc.sbuf_tensor("%s_%d" % (name, self.uid), list(shape), dt))
        return Buf(name, t)

    def ps(self, name, shape, dt=F32):
        t = self.stacks[-1].enter_context(self.nc.psum_tensor(name, list(shape), dt))
        return Buf(name, t)

    def view(self, buf, name=None):
        return Buf(name or buf.name + "_v", buf.t)

    def _wait(self, eng, tok):
        if tok is None:
            return
        sem, val = tok
        key = id(sem)
        if eng.seen.get(key, 0) >= val:
            return
        eng.h.wait_ge(sem, val)
        eng.seen[key] = val

    def _deps(self, eng, reads, writes):
        own = id(eng.sem)
        for b in reads:
            self._wait(eng, b.lw)
            if b.excl:
                for tok in b.rd:
                    if id(tok[0]) != own:
                        self._wait(eng, tok)
        for b in writes:
            if b.lw is not None and id(b.lw[0]) != own:
                self._wait(eng, b.lw)
            for tok in b.rd:
                if id(tok[0]) != own:
                    self._wait(eng, tok)

    def _commit(self, tok, reads, writes):
        for b in reads:
            b.rd.append(tok)
            if len(b.rd) > 48:
                d = {}
                for s, v in b.rd:
                    k = id(s)
                    if k not in d or d[k][1] < v:
                        d[k] = (s, v)
                b.rd = list(d.values())
        for b in writes:
            b.lw = tok
            b.rd = []

    def op(self, en, fn, reads=(), writes=()):
        eng = self.E[en]
        self._deps(eng, reads, writes)
        ins = fn(eng.h)
        eng.cnt += 1
        ins.then_inc(eng.sem, 1)
        tok = (eng.sem, eng.cnt)
        self._commit(tok, reads, writes)
        return tok

    def dma(self, en, out, in_, reads=(), writes=(), **kw):
        eng = self.E[en]
        self._deps(eng, reads, writes)
        kind = "sw" if en == "pool" else "hw"
        pool_ = self.dma_pools[kind]
        slot = pool_[self.dma_rr[kind]]
        self.dma_rr[kind] = (self.dma_rr[kind] + 1) % len(pool_)
        sem, val = slot
        if val:
            self._wait(eng, (sem, val))
        ins = eng.h.dma_start(out=out, in_=in_, **kw)
        slot[1] = val + 16
        ins.then_inc(sem, 16)
        tok = (sem, val + 16)
        self._commit(tok, reads, writes)
        return tok

    def barrier(self):
        for e in self.E.values():
            for f in self.E.values():
                if f is not e and f.cnt:
                    self._wait(e, (f.sem, f.cnt))
            for sem, val in self.dma_sems:
                if val:
                    self._wait(e, (sem, val))

    def close(self):
        while self.stacks:
            self.stacks.pop().close()


def bc(ap, shape):
    return ap.to_broadcast(list(shape))


def t5_bucket_np(n):
    n = np.maximum(n, 0).astype(np.int32)
    nf = np.maximum(n, 1).astype(np.float32)
    large = 16 + (np.log(nf / np.float32(16)) / np.float32(math.log(128 / 16)) * np.float32(16)).astype(np.int32)
    large = np.minimum(large, 31)
    return np.where(n < 16, n, large)


def bucket_onehot():
    oh = np.zeros((32, 384), np.float32)
    n = np.arange(384) - 127
    b = t5_bucket_np(n)
    for m_ in range(384):
        if n[m_] >= 0:
            oh[b[m_], m_] = 1.0
    return oh


def bucket_lohi():
    b = t5_bucket_np(np.arange(0, 4096))
    lo = np.zeros(32, np.float32)
    hi = np.zeros(32, np.float32)
    for k in range(32):
        idx = np.nonzero(b == k)[0]
        lo[k] = idx.min()
        hi[k] = idx.max() + 1
    hi[31] = 1.0e9
    return np.concatenate([lo, hi]).astype(np.float32)


def build(stop_after=None, dbg=()):
    nc = bass.Bass("TRN2", target_bir_lowering=False)

    def din(name, shape, dt=F32):
        return nc.dram_tensor(name, list(shape), dt, kind="ExternalInput").ap()

    x_d = din("x", [T, D])
    c_d = din("c_fm", [128, 8])
    ada_w = din("ada_w", [D, 6 * D])
    ada_b = din("ada_b", [6 * D])
    n1g = din("norm1_g", [D])
    n2g = din("norm2_g", [D])
    w_in = din("w_in", [D, INW])
    qng = din("q_norm_g", [64])
    kng = din("k_norm_g", [64])
    kvng = din("kv_norm_g", [128])
    wkv = din("w_kv_up", [128, 128])
    ikng = din("idx_k_norm_g", [32])
    relb = din("rel_bias", [256])
    cw_d = din("conv_w_fm", [128, 12, 4])
    alog = din("a_log", [4])
    dtb = din("dt_bias", [4])
    dng = din("dn_norm_g", [128])
    w_out = din("w_out", [D, D])
    rw_d = din("router_w", [D, 36])
    rb_d = din("router_b", [36])
    w1 = din("w1", [32, D, 512])
    w3 = din("w3", [32, D, 512])
    w2 = din("w2", [32, 512, D])
    ohn_d = din("bk_ohn", [32, 384])
    out_d = nc.dram_tensor("out", [T, D], F32, kind="ExternalOutput").ap()
    dbg_d = {}

    c = Ctx(nc)
    op, dma = c.op, c.dma
    w_in_v = w_in.rearrange("(j p) n -> p j n", p=128)

    PT = [c.ps("ps%d" % i, [128, 1024]) for i in range(4)]
    PB = []
    for i in range(8):
        PB.append(c.view(PT[i // 2], "bank%d" % i))
        PB[-1].excl = True

    def bank(i, rows=128, c0=0, c1=512):
        return PT[i // 2].t[0:rows, (i % 2) * 512 + c0:(i % 2) * 512 + c1]

    def bank_bf(i, rows=128, n=1024):
        return PT[i // 2].t[0:rows, (i % 2) * 512:(i % 2) * 512 + 512].bitcast(BF16)[:, 0:n]

    identF = c.sb("identF", [128, 128])
    identB = c.sb("identB", [128, 128], BF16)
    onesF = c.sb("onesF", [128, 128])
    epsT = c.sb("epsT", [128, 1])
    op("pool", lambda h: h.memset(onesF[:], 1.0), writes=[onesF])
    op("pool", lambda h: h.memset(epsT[:], EPS), writes=[epsT])
    op("pool", lambda h: h.memset(identF[:], 1.0), writes=[identF])
    op("pool", lambda h: h.affine_select(out=identF[:], in_=identF[:], pattern=[[-1, 128]], compare_op=ALU.is_equal,
                                         fill=0.0, base=0, channel_multiplier=1), reads=[identF], writes=[identF])
    op("dve", lambda h: h.tensor_copy(out=identB[:], in_=identF[:]), reads=[identF], writes=[identB])

    MODBC = c.sb("MODBC", [128, 6 * D])
    SH1, A1, GT1, SH2, A2, GT2 = [MODBC[:, k * D:(k + 1) * D] for k in range(6)]
    HT = c.sb("HT", [128, 8, T], BF16)
    HTv = [c.view(HT, "HT%d" % i) for i in range(NT)]

    def rstd_from_ss(ss_ap, out_ap, n, reads, writes_buf, tmp_buf, tmp_ap):
        op("act", lambda h: h.activation(out=tmp_ap, in_=ss_ap, func=AF.Sqrt, bias=epsT[0:tmp_ap.shape[0], :], scale=1.0 / n),
           reads=reads + [epsT], writes=[tmp_buf])
        op("dve", lambda h: h.reciprocal(out=out_ap, in_=tmp_ap), reads=[tmp_buf], writes=[writes_buf])

    c.push()
    cs = c.sb("cs", [128, 8])
    SCR = c.sb("SCR", [128, 8, 128])
    G1BC = c.sb("G1BC", [128, D])
    G2BC = c.sb("G2BC", [128, D])
    AW = [c.sb("AW%d" % k, [128, 8, 512]) for k in range(2)]
    MODv = [c.view(MODBC, "MOD%d" % k) for k in range(6)]
    dma("sp", cs[:], c_d, writes=[cs])
    dma("sp", MODBC[:], ada_b.partition_broadcast(128), writes=MODv)
    dma("sp", G1BC[:], n1g.partition_broadcast(128), writes=[G1BC])
    dma("sp", G2BC[:], n2g.partition_broadcast(128), writes=[G2BC])
    op("act", lambda h: h.activation(out=cs[:], in_=cs[:], func=AF.Silu), reads=[cs], writes=[cs])
    for j in range(8):
        op("dve", lambda h: h.tensor_scalar(out=SCR[:, j, :], in0=onesF[:], scalar1=cs[:, j:j + 1], scalar2=None, op0=ALU.mult),
           reads=[onesF, cs], writes=[SCR])
    ada_v = ada_w.rearrange("(j p) n -> p j n", p=128)

    def mod_piece(m):
        aw = AW[m % 2]
        mv = MODv[m // 2]
        dma("sp", aw[:], ada_v[:, :, m * 512:(m + 1) * 512], writes=[aw])
        b = 2 + m % 2
        for j in range(8):
            op("pe", lambda h: h.matmul(bank(b), lhsT=SCR[:, j, :], rhs=aw[:, j, :], start=(j == 0), stop=(j == 7)),
               reads=[SCR, aw], writes=[PB[b]])
        op("dve", lambda h: h.tensor_tensor(out=MODBC[:, m * 512:(m + 1) * 512], in0=bank(b), in1=MODBC[:, m * 512:(m + 1) * 512], op=ALU.add),
           reads=[PB[b], mv], writes=[mv])
        if m == 3:
            op("dve", lambda h: h.scalar_tensor_tensor(out=A1, in0=A1, scalar=1.0, in1=G1BC[:], op0=ALU.add, op1=ALU.mult),
               reads=[MODv[1], G1BC], writes=[MODv[1]])
        if m == 9:
            op("dve", lambda h: h.scalar_tensor_tensor(out=A2, in0=A2, scalar=1.0, in1=G2BC[:], op0=ALU.add, op1=ALU.mult),
               reads=[MODv[4], G2BC], writes=[MODv[4]])

    for m in range(4):
        mod_piece(m)

    def norm_front(xt_buf, xt_ap, i, A_ap, S_ap, ss_buf, tmpA, tmpB, hb, extra_reads=(), modb=None):
        modb = modb or [MODBC]
        junk = tmpA
        op("act", lambda h: h.activation(out=junk[:], in_=xt_ap, func=AF.Square, accum_out=ss_buf[:, 0:1]),
           reads=[xt_buf] + list(extra_reads), writes=[junk, ss_buf])
        rstd_from_ss(ss_buf[:, 0:1], ss_buf[:, 2:3], D, [ss_buf], ss_buf, ss_buf, ss_buf[:, 1:2])
        op("dve", lambda h: h.scalar_tensor_tensor(out=tmpB[:], in0=xt_ap, scalar=ss_buf[:, 2:3], in1=A_ap, op0=ALU.mult, op1=ALU.mult),
           reads=[xt_buf, ss_buf] + modb + list(extra_reads), writes=[tmpB])
        op("pool", lambda h: h.tensor_tensor(out=hb[:], in0=tmpB[:], in1=S_ap, op=ALU.add), reads=[tmpB] + modb, writes=[hb])

    def norm_back(i, hb, pbank):
        pb = bank_bf(pbank)
        for k in range(8):
            op("pe", lambda h: h.transpose(out=pb[:, k * 128:(k + 1) * 128], in_=hb[:, k * 128:(k + 1) * 128], identity=identB[:]),
               reads=[hb, identB], writes=[PB[pbank]])
        op("act", lambda h: h.activation(out=HT[:, :, i * 128:(i + 1) * 128], in_=pb.rearrange("p (k t) -> p k t", t=128), func=AF.Copy),
           reads=[PB[pbank]], writes=[HTv[i]])

    def norm_to_HT(xt_buf, xt_ap, i, A_ap, S_ap, ss_buf, tmpA, tmpB, hb, pbank, extra_reads=()):
        norm_front(xt_buf, xt_ap, i, A_ap, S_ap, ss_buf, tmpA, tmpB, hb, extra_reads)
        norm_back(i, hb, pbank)

    NR = 3
    XS = [c.sb("XS%d" % k, [128, D]) for k in range(NR)]
    SS = [c.sb("SS%d" % k, [128, 4]) for k in range(NR)]
    TA = c.sb("TA", [128, D])
    TB = [c.sb("TB%d" % k, [128, D]) for k in range(NR)]
    HB = [c.sb("HB%d" % k, [128, D], BF16) for k in range(NR)]

    def s2_front(i):
        xs = XS[i % NR]
        dma("sp", xs[:], x_d[i * 128:(i + 1) * 128, :], writes=[xs])
        norm_front(xs, xs[:], i, A1, SH1, SS[i % NR], TA, TB[i % NR], HB[i % NR], modb=[MODv[0], MODv[1]])

    s2_front(0)
    for i in range(NT):
        if i + 1 < NT:
            s2_front(i + 1)
        norm_back(i, HB[i % NR], i % 2)
        if i % 2 == 1 and 4 + i // 2 < 12:
            mod_piece(4 + i // 2)
    c.pop()
    if "HT" in dbg:
        dbg_d["HT"] = (HT, nc.dram_tensor("dbg_HT", [128, 8 * T], BF16, kind="ExternalOutput").ap())

    c.push()
    DNT = c.sb("DNT", [128, 4, T], BF16)
    if "nogdn" in dbg:
        op("pool", lambda h: h.memset(DNT[:], 0.0), writes=[DNT])
    elif stop_after != "s2":
        gdn_stage(c, nc, locals())
    if "DNT" in dbg:
        dbg_d["DNT"] = (DNT, nc.dram_tensor("dbg_DNT", [128, 4 * T], BF16, kind="ExternalOutput").ap())
    if stop_after in ("s2", "gdn", "gdn1"):
        return finish(c, nc, dbg_d, out_d, None)

    attn_stage(c, nc, locals())
    if stop_after in ("attn", "attn1"):
        return finish(c, nc, dbg_d, out_d, None)
    c.pop()
    n_exp = 32 if "e2" not in dbg else 2
    moe_stage(c, nc, locals())
    return finish(c, nc, dbg_d, out_d, None)


def finish(c, nc, dbg_d, out_d, X):
    for name, (buf, dap) in dbg_d.items():
        shape = dap.shape
        c.dma("sp", dap, buf.t[:].rearrange("p a b -> p (a b)") if len(buf.t.shape) == 3 else buf.t[:], reads=[buf])
    c.barrier()
    c.close()
    return nc


def gdn_stage(c, nc, L):
    op, dma, bank, PB, PT = L["op"], L["dma"], L["bank"], L["PB"], L["PT"]
    identF, onesF, epsT, HT, HTv, DNT, w_in_v = L["identF"], L["onesF"], L["epsT"], L["HT"], L["HTv"], L["DNT"], L["w_in_v"]
    cw_d, alog, dtb, dng, dbg, dbg_d = L["cw_d"], L["alog"], L["dtb"], L["dng"], L["dbg"], L["dbg_d"]
    c.push()

    def pbs(q):
        return [PB[2 * q], PB[2 * q + 1]]

    def P3(q, rows, inner):
        return PT[q].t[0:rows, :].rearrange("p (n d) -> p n d", d=inner)

    TUi = c.sb("TUi", [64, 64]); TUs = c.sb("TUs", [64, 64]); TLs = c.sb("TLs", [64, 64]); selL = c.sb("selL", [64, 128])
    for t_, pat, cm, base, cmp_ in ((TUi, [[1, 64]], -1, 0, ALU.is_ge), (TUs, [[1, 64]], -1, -1, ALU.is_ge),
                                   (TLs, [[-1, 64]], 1, -1, ALU.is_ge), (selL, [[0, 128]], 1, -63, ALU.is_equal)):
        op("pool", lambda h: h.memset(t_[:], 1.0), writes=[t_])
        op("pool", lambda h: h.affine_select(out=t_[:], in_=t_[:], pattern=pat, compare_op=cmp_, fill=0.0, base=base,
                                             channel_multiplier=cm), reads=[t_], writes=[t_])
    I64 = identF[0:64, 0:64]
    CW = c.sb("CW", [128, 12, 4]); dma("sp", CW[:], cw_d, writes=[CW])
    DNG = c.sb("DNG", [64, 128]); dma("sp", DNG[:], dng.partition_broadcast(64), writes=[DNG])
    DTB = c.sb("DTB", [64, 4]); dma("sp", DTB[:], dtb.partition_broadcast(64), writes=[DTB])
    NA = c.sb("NA", [64, 4]); dma("sp", NA[:], alog.partition_broadcast(64), writes=[NA])
    op("act", lambda h: h.activation(out=NA[:], in_=NA[:], func=AF.Exp), reads=[NA], writes=[NA])
    op("dve", lambda h: h.tensor_scalar(out=NA[:], in0=NA[:], scalar1=-1.0, scalar2=None, op0=ALU.mult), reads=[NA], writes=[NA])

    WBA = c.sb("WBA", [128, 8, 8], BF16)
    dma("pool", WBA[:], w_in_v[:, :, C_BETA:C_BETA + 8], writes=[WBA])
    for n in range(32):
        for k in range(8):
            op("pe", lambda h: h.matmul(bank(0, 64, 8 * n, 8 * n + 8), lhsT=HT[:, k, 64 * n:64 * n + 64], rhs=WBA[:, k, :],
                                        start=(k == 0), stop=(k == 7)), reads=[HTv[n // 2], WBA], writes=[PB[0]])
    BAR = c.sb("BAR", [64, 32, 8]); BETA = c.sb("BETA", [64, 32, 4]); GG = c.sb("GG", [64, 32, 4])
    GC = c.sb("GC", [64, 32, 4]); EG = c.sb("EG", [64, 32, 4]); BEG = c.sb("BEG", [64, 32, 4])
    GCB = c.sb("GCB", [128, 32, 4]); EKD = c.sb("EKD", [64, 32, 4])
    op("act", lambda h: h.activation(out=BAR[:], in_=bank(0, 64, 0, 256).rearrange("p (n e) -> p n e", e=8), func=AF.Copy),
       reads=[PB[0]], writes=[BAR])
    op("act", lambda h: h.activation(out=BETA[:], in_=BAR[:, :, 0:4], func=AF.Sigmoid), reads=[BAR], writes=[BETA])
    op("dve", lambda h: h.tensor_tensor(out=GG[:], in0=BAR[:, :, 4:8], in1=bc(DTB[:].unsqueeze(1), [64, 32, 4]), op=ALU.add),
       reads=[BAR, DTB], writes=[GG])
    op("act", lambda h: h.activation(out=GG[:], in_=GG[:], func=AF.Exp), reads=[GG], writes=[GG])
    op("act", lambda h: h.activation(out=GG[:], in_=GG[:], func=AF.Ln, bias=1.0, scale=1.0), reads=[GG], writes=[GG])
    op("dve", lambda h: h.tensor_tensor(out=GG[:], in0=GG[:], in1=bc(NA[:].unsqueeze(1), [64, 32, 4]), op=ALU.mult),
       reads=[GG, NA], writes=[GG])
    GGf = GG[:].rearrange("p n h -> p (n h)")
    GCf = GC[:].rearrange("p n h -> p (n h)")
    op("pe", lambda h: h.matmul(bank(1, 64, 0, 128), lhsT=TUi[:], rhs=GGf, start=True, stop=True), reads=[TUi, GG], writes=[PB[1]])
    op("act", lambda h: h.activation(out=GCf, in_=bank(1, 64, 0, 128), func=AF.Copy), reads=[PB[1]], writes=[GC])
    op("act", lambda h: h.activation(out=EG[:], in_=GC[:], func=AF.Exp), reads=[GC], writes=[EG])
    op("dve", lambda h: h.tensor_tensor(out=BEG[:], in0=BETA[:], in1=EG[:], op=ALU.mult), reads=[BETA, EG], writes=[BEG])
    op("pe", lambda h: h.matmul(bank(2, 128, 0, 128), lhsT=selL[:], rhs=GCf, start=True, stop=True), reads=[selL, GC], writes=[PB[2]])
    op("act", lambda h: h.activation(out=GCB[:].rearrange("p n h -> p (n h)"), in_=bank(2, 128, 0, 128), func=AF.Exp),
       reads=[PB[2]], writes=[GCB])
    op("dve", lambda h: h.tensor_tensor(out=EKD[:].rearrange("p n h -> p (n h)"), in0=bank(2, 64, 0, 128), in1=GCf, op=ALU.subtract),
       reads=[PB[2], GC], writes=[EKD])
    op("act", lambda h: h.activation(out=EKD[:], in_=EKD[:], func=AF.Exp), reads=[EKD], writes=[EKD])

    NB = 8
    NTK = 64 * NB
    NQB = T // NTK

    def make_set(hs):
        B0 = 4 * hs
        PA, PBk = PT[2 * hs], PT[2 * hs + 1]
        pbA = [PB[B0], PB[B0 + 1]]
        pbB = [PB[B0 + 2], PB[B0 + 3]]
        sfx = "_%d" % hs
        WG = c.sb("WG" + sfx, [128, 8, 512], BF16)
        FRAW = c.sb("FRAW" + sfx, [128, NTK + 3])
        CV = [c.sb("CV%d%s" % (g, sfx), [128, NTK]) for g in range(3)]
        SZ = c.sb("SZ" + sfx, [128, NTK])
        BK = c.sb("BK" + sfx, [64, NB, 128]); KD = c.sb("KD" + sfx, [64, NB, 128]); BV = c.sb("BV" + sfx, [64, NB, 128])
        R = c.sb("R" + sfx, [64, NB, 64]); D1 = c.sb("D1" + sfx, [128, NTK]); E1 = c.sb("E1" + sfx, [128, NTK]); E2 = c.sb("E2" + sfx, [128, NTK])
        PTS = c.sb("PTS" + sfx, [64, NB, 64]); Tt = c.sb("Tt" + sfx, [64, NB, 64]); Mn = c.sb("Mn" + sfx, [64, NB, 64]); An = c.sb("An" + sfx, [64, NB, 64])
        U = c.sb("U" + sfx, [64, NB, 128]); WT = c.sb("WT" + sfx, [128, NTK]); O = c.sb("O" + sfx, [64, NB, 128]); OJ = c.sb("OJ" + sfx, [64, 128])
        Sst = [c.sb("Sst%d%s" % (k, sfx), [128, 128]) for k in range(2)]
        DL = [c.sb("DL%d%s" % (k, sfx), [64, 128]) for k in range(2)]
        MS = c.sb("MS" + sfx, [64, NB]); MS2 = c.sb("MS2" + sfx, [64, NB])
        v3 = lambda buf: buf[0:64, :].rearrange("p (n d) -> p n d", d=64)
        I64b = bc(I64.unsqueeze(1), [64, NB, 64])
        Rf = R[:].rearrange("p n s -> p (n s)")
        state = {"scur": 0}
        A3 = lambda inner: PA.t[0:64, 0:NB * inner].rearrange("p (n d) -> p n d", d=inner) if NB * inner <= 1024 else None
        bA0 = lambda rows, c0, c1: PA.t[0:rows, c0:c1]
        bA1 = lambda rows, c0, c1: PA.t[0:rows, 512 + c0:512 + c1]
        bB0 = lambda rows, c0, c1: PBk.t[0:rows, c0:c1]
        bB1 = lambda rows, c0, c1: PBk.t[0:rows, 512 + c0:512 + c1]
        w64 = lambda f: f(64, 0, 64 * NB).rearrange("p (n d) -> p n d", d=64)

        def batch(hd, qb):
            th = []
            t0 = NTK * qb
            n0 = NB * qb
            nsl = slice(n0, n0 + NB)
            tiles = [HTv[i] for i in range(max(0, (t0 - 3) // 128), (t0 + NTK) // 128)]
            QN, KN, VN = CV
            if qb == 0:
                def p_w():
                    cols = [C_DNQKV + hd * 128, C_DNQKV + 512 + hd * 128, C_DNQKV + 1024 + hd * 128, C_DNZ + hd * 128]
                    for gi, c0 in enumerate(cols):
                        dma("pool", WG[:, :, gi * 128:(gi + 1) * 128], w_in_v[:, :, c0:c0 + 128], writes=[WG])
                    state["scur"] = 0
                    op("pool", lambda h: h.memset(Sst[0][:], 0.0), writes=[Sst[0]])
                th.append(p_w)
            for g in range(3):
                def p_proj(g=g):
                    ch = g * 4 + hd
                    cv = CV[g]
                    if qb == 0:
                        op("pool", lambda h: h.memset(FRAW[:, 0:3], 0.0), writes=[FRAW])
                    else:
                        for k in range(8):
                            op("pe", lambda h: h.matmul(bA1(128, 0, 3), lhsT=WG[:, k, g * 128:(g + 1) * 128], rhs=HT[:, k, t0 - 3:t0],
                                                        start=(k == 0), stop=(k == 7)), reads=[WG] + tiles, writes=[pbA[1]])
                        op("act", lambda h: h.activation(out=FRAW[:, 0:3], in_=bA1(128, 0, 3), func=AF.Copy), reads=[pbA[1]], writes=[FRAW])
                    for k in range(8):
                        op("pe", lambda h: h.matmul(bA0(128, 0, NTK), lhsT=WG[:, k, g * 128:(g + 1) * 128], rhs=HT[:, k, t0:t0 + NTK], start=(k == 0), stop=(k == 7)),
                           reads=[WG] + tiles, writes=[pbA[0]])
                    op("act", lambda h: h.activation(out=FRAW[:, 3:3 + NTK], in_=bA0(128, 0, NTK), func=AF.Copy), reads=[pbA[0]], writes=[FRAW])
                    op("dve", lambda h: h.tensor_scalar(out=cv[:], in0=FRAW[:, 3:3 + NTK], scalar1=CW[:, ch, 3:4], scalar2=None, op0=ALU.mult), reads=[FRAW, CW], writes=[cv])
                    for j in (2, 1, 0):
                        op("dve", lambda h: h.scalar_tensor_tensor(out=cv[:], in0=FRAW[:, j:j + NTK], scalar=CW[:, ch, j:j + 1], in1=cv[:],
                                                                   op0=ALU.mult, op1=ALU.add), reads=[FRAW, CW, cv], writes=[cv])
                    op("act", lambda h: h.activation(out=cv[:], in_=cv[:], func=AF.Silu), reads=[cv], writes=[cv])
                th.append(p_proj)

            def p_z():
                for k in range(8):
                    op("pe", lambda h: h.matmul(bA0(128, 0, NTK), lhsT=WG[:, k, 384:512], rhs=HT[:, k, t0:t0 + NTK], start=(k == 0), stop=(k == 7)), reads=[WG] + tiles, writes=[pbA[0]])
                op("act", lambda h: h.activation(out=SZ[:], in_=bA0(128, 0, NTK), func=AF.Silu), reads=[pbA[0]], writes=[SZ])
            th.append(p_z)
            for g in range(2):
                def p_l2(g=g):
                    cv = CV[g]
                    op("pool", lambda h: h.tensor_tensor(out=D1[:], in0=cv[:], in1=cv[:], op=ALU.mult), reads=[cv], writes=[D1])
                    op("pe", lambda h: h.matmul(bA1(128, 0, NTK), lhsT=onesF[:], rhs=D1[:], start=True, stop=True), reads=[onesF, D1], writes=[pbA[1]])
                    op("act", lambda h: h.activation(out=E1[:], in_=bA1(128, 0, NTK), func=AF.Sqrt, bias=epsT[:], scale=1.0), reads=[pbA[1], epsT], writes=[E1])
                    op("dve", lambda h: h.reciprocal(out=E1[:], in_=E1[:]), reads=[E1], writes=[E1])
                    sc_ = 128.0 ** -0.5 if g == 0 else 1.0
                    op("dve", lambda h: h.scalar_tensor_tensor(out=cv[:], in0=E1[:], scalar=sc_, in1=cv[:], op0=ALU.mult, op1=ALU.mult), reads=[E1, cv], writes=[cv])
                th.append(p_l2)

            def p_kv():
                for n_ in range(NB):
                    op("pe", lambda h: h.transpose(out=PA.t[0:64, n_ * 128:n_ * 128 + 128], in_=KN[:, 64 * n_:64 * n_ + 64], identity=identF[:]),
                       reads=[KN, identF], writes=[pbA[n_ // 4]])
                    op("pe", lambda h: h.transpose(out=PBk.t[0:64, n_ * 128:n_ * 128 + 128], in_=VN[:, 64 * n_:64 * n_ + 64], identity=identF[:]),
                       reads=[VN, identF], writes=[pbB[n_ // 4]])
                pk = PA.t[0:64, :].rearrange("p (n d) -> p n d", d=128)
                pv = PBk.t[0:64, :].rearrange("p (n d) -> p n d", d=128)
                op("dve", lambda h: h.tensor_tensor(out=BK[:], in0=pk, in1=bc(BEG[:, nsl, hd:hd + 1], [64, NB, 128]), op=ALU.mult), reads=pbA + [BEG], writes=[BK])
                op("dve", lambda h: h.tensor_tensor(out=KD[:], in0=pk, in1=bc(EKD[:, nsl, hd:hd + 1], [64, NB, 128]), op=ALU.mult), reads=pbA + [EKD], writes=[KD])
                op("dve", lambda h: h.tensor_tensor(out=BV[:], in0=pv, in1=bc(BETA[:, nsl, hd:hd + 1], [64, NB, 128]), op=ALU.mult), reads=pbB + [BETA], writes=[BV])
            th.append(p_kv)
            d1 = v3(D1); e1 = v3(E1); e2 = v3(E2)

            def p_kk():
                for n_ in range(NB):
                    sl = slice(64 * n_, 64 * n_ + 64)
                    op("pe", lambda h: h.matmul(bA0(64, 64 * n_, 64 * n_ + 64), lhsT=KN[:, sl], rhs=KN[:, sl], start=True, stop=True), reads=[KN], writes=[pbA[0]])
                    op("pe", lambda h: h.matmul(bA1(64, 64 * n_, 64 * n_ + 64), lhsT=KN[:, sl], rhs=QN[:, sl], start=True, stop=True), reads=[KN, QN], writes=[pbA[1]])
                op("dve", lambda h: h.tensor_tensor(out=R[:], in0=bc(GC[:, nsl, hd:hd + 1], [64, NB, 64]), in1=I64b, op=ALU.mult), reads=[GC, identF], writes=[R])
                op("pe", lambda h: h.matmul(bB0(64, 0, NTK), lhsT=onesF[0:64, 0:64], rhs=Rf, start=True, stop=True), reads=[onesF, R], writes=[pbB[0]])
                op("dve", lambda h: h.tensor_tensor(out=d1, in0=w64(bB0), in1=bc(GC[:, nsl, hd:hd + 1], [64, NB, 64]), op=ALU.subtract), reads=[pbB[0], GC], writes=[D1])
                op("dve", lambda h: h.tensor_tensor(out=R[:], in0=bc(BETA[:, nsl, hd:hd + 1], [64, NB, 64]), in1=I64b, op=ALU.mult), reads=[BETA, identF], writes=[R])
                op("pe", lambda h: h.matmul(bB1(64, 0, NTK), lhsT=onesF[0:64, 0:64], rhs=Rf, start=True, stop=True), reads=[onesF, R], writes=[pbB[1]])
                op("dve", lambda h: h.tensor_scalar(out=e1, in0=d1, scalar1=0.0, scalar2=None, op0=ALU.min), reads=[D1], writes=[E1])
                op("dve", lambda h: h.tensor_scalar(out=e2, in0=d1, scalar1=-1.0, scalar2=0.0, op0=ALU.mult, op1=ALU.min), reads=[D1], writes=[E2])
                op("act", lambda h: h.activation(out=e1, in_=e1, func=AF.Exp), reads=[E1], writes=[E1])
                op("act", lambda h: h.activation(out=e2, in_=e2, func=AF.Exp), reads=[E2], writes=[E2])
            th.append(p_kk)

            def p_am():
                op("dve", lambda h: h.tensor_tensor(out=PTS[:], in0=w64(bA1), in1=e1, op=ALU.mult), reads=[pbA[1], E1], writes=[PTS])
                op("pool", lambda h: h.tensor_tensor(out=PTS[:], in0=PTS[:], in1=bc(TUi[:].unsqueeze(1), [64, NB, 64]), op=ALU.mult), reads=[PTS, TUi], writes=[PTS])
                op("dve", lambda h: h.tensor_tensor(out=e1, in0=w64(bA0), in1=e1, op=ALU.mult), reads=[pbA[0], E1], writes=[E1])
                op("pool", lambda h: h.tensor_tensor(out=e1, in0=e1, in1=bc(TUs[:].unsqueeze(1), [64, NB, 64]), op=ALU.mult), reads=[E1, TUs], writes=[E1])
                op("dve", lambda h: h.scalar_tensor_tensor(out=e1, in0=e1, scalar=-1.0, in1=w64(bB1), op0=ALU.mult, op1=ALU.mult), reads=[E1, pbB[1]], writes=[E1])
                op("dve", lambda h: h.tensor_tensor(out=e2, in0=w64(bA0), in1=e2, op=ALU.mult), reads=[pbA[0], E2], writes=[E2])
                op("pool", lambda h: h.tensor_tensor(out=e2, in0=e2, in1=bc(TLs[:].unsqueeze(1), [64, NB, 64]), op=ALU.mult), reads=[E2, TLs], writes=[E2])
                op("dve", lambda h: h.scalar_tensor_tensor(out=e2, in0=e2, scalar=-1.0, in1=bc(BETA[:, nsl, hd:hd + 1], [64, NB, 64]), op0=ALU.mult, op1=ALU.mult),
                   reads=[E2, BETA], writes=[E2])
                op("pool", lambda h: h.tensor_tensor(out=Tt[:], in0=e1, in1=I64b, op=ALU.add), reads=[E1, identF], writes=[Tt])
            th.append(p_am)
            cur = {"Ac": e1, "Mc": e2, "Acb": E1, "Mcb": E2, "Anx": An[:], "Mnx": Mn[:], "Anb": An, "Mnb": Mn}
            for lvl in range(1, 6):
                def p_lvl(lvl=lvl):
                    Ac, Mc, Acb, Mcb, Anx, Mnx, Anb, Mnb = (cur[k] for k in ("Ac", "Mc", "Acb", "Mcb", "Anx", "Mnx", "Anb", "Mnb"))
                    for n_ in range(NB):
                        op("pe", lambda h: h.matmul(bA0(64, 64 * n_, 64 * n_ + 64), lhsT=Ac[:, n_, :], rhs=Mc[:, n_, :], start=True, stop=True), reads=[Acb, Mcb], writes=[pbA[0]])
                    if lvl < 5:
                        for n_ in range(NB):
                            op("pe", lambda h: h.matmul(bA1(64, 64 * n_, 64 * n_ + 64), lhsT=Mc[:, n_, :], rhs=Ac[:, n_, :], start=True, stop=True), reads=[Acb, Mcb], writes=[pbA[1]])
                    op("act", lambda h: h.activation(out=Mnx, in_=w64(bA0), func=AF.Copy), reads=[pbA[0]], writes=[Mnb])
                    if lvl < 5:
                        op("dve", lambda h: h.tensor_copy(out=Anx, in_=w64(bA1)), reads=[pbA[1]], writes=[Anb])
                    for n_ in range(NB):
                        op("pe", lambda h: h.matmul(bB0(64, 64 * n_, 64 * n_ + 64), lhsT=Mnx[:, n_, :], rhs=Tt[:, n_, :], start=True, stop=True), reads=[Mnb, Tt], writes=[pbB[0]])
                    op("dve", lambda h: h.tensor_tensor(out=Tt[:], in0=Tt[:], in1=w64(bB0), op=ALU.add), reads=[Tt, pbB[0]], writes=[Tt])
                    cur.update(Ac=Anx, Mc=Mnx, Acb=Anb, Mcb=Mnb, Anx=Ac, Mnx=Mc, Anb=Acb, Mnb=Mcb)
                th.append(p_lvl)

            def p_sol():
                for n_ in range(NB):
                    op("pe", lambda h: h.matmul(PA.t[0:64, n_ * 128:n_ * 128 + 128], lhsT=Tt[:, n_, :], rhs=BV[:, n_, :], start=True, stop=True), reads=[Tt, BV], writes=[pbA[n_ // 4]])
                    op("pe", lambda h: h.matmul(bB0(128, 64 * n_, 64 * n_ + 64), lhsT=BK[:, n_, :], rhs=Tt[:, n_, :], start=True, stop=True), reads=[Tt, BK], writes=[pbB[0]])
                op("act", lambda h: h.activation(out=U[:], in_=PA.t[0:64, :].rearrange("p (n d) -> p n d", d=128), func=AF.Copy), reads=pbA, writes=[U])
                op("dve", lambda h: h.tensor_copy(out=WT[:], in_=bB0(128, 0, NTK)), reads=[pbB[0]], writes=[WT])
                op("dve", lambda h: h.tensor_tensor(out=R[:], in0=bc(EG[:, nsl, hd:hd + 1], [64, NB, 64]), in1=I64b, op=ALU.mult), reads=[EG, identF], writes=[R])
                op("pe", lambda h: h.matmul(bB1(128, 0, NTK), lhsT=onesF[0:64, :], rhs=Rf, start=True, stop=True), reads=[onesF, R], writes=[pbB[1]])
                op("dve", lambda h: h.tensor_tensor(out=QN[:], in0=QN[:], in1=bB1(128, 0, NTK), op=ALU.mult), reads=[QN, pbB[1]], writes=[QN])
            th.append(p_sol)
            for n_ in range(NB):
                def q_step(n_=n_):
                    n = n0 + n_
                    scur = state["scur"]
                    S0, S1 = Sst[scur], Sst[1 - scur]
                    dl = DL[n_ % 2]
                    sl = slice(64 * n_, 64 * n_ + 64)
                    op("pe", lambda h: h.matmul(bA0(64, 0, 128), lhsT=WT[:, sl], rhs=S0[:], start=True, stop=True), reads=[WT, S0], writes=[pbA[0]])
                    op("dve", lambda h: h.tensor_tensor(out=dl[:], in0=U[:, n_, :], in1=bA0(64, 0, 128), op=ALU.subtract), reads=[U, pbA[0]], writes=[dl])
                    op("pe", lambda h: h.matmul(bA1(64, 0, 128), lhsT=QN[:, sl], rhs=S0[:], start=True, stop=False), reads=[QN, S0], writes=[pbA[1]])
                    op("pe", lambda h: h.matmul(bA1(64, 0, 128), lhsT=PTS[:, n_, :], rhs=dl[:], start=False, stop=True), reads=[PTS, dl], writes=[pbA[1]])
                    op("act", lambda h: h.activation(out=O[:, n_, :], in_=bA1(64, 0, 128), func=AF.Copy), reads=[pbA[1]], writes=[O])
                    op("pe", lambda h: h.matmul(bB0(128, 0, 128), lhsT=KD[:, n_, :], rhs=dl[:], start=True, stop=True), reads=[KD, dl], writes=[pbB[0]])
                    op("dve", lambda h: h.scalar_tensor_tensor(out=S1[:], in0=S0[:], scalar=GCB[:, n, hd:hd + 1], in1=bB0(128, 0, 128), op0=ALU.mult, op1=ALU.add),
                       reads=[S0, GCB, pbB[0]], writes=[S1])
                    op("act", lambda h: h.activation(out=OJ[:], in_=O[:, n_, :], func=AF.Square, accum_out=MS[:, n_:n_ + 1]), reads=[O], writes=[OJ, MS])
                    state["scur"] = 1 - scur
                th.append(q_step)

            def q_out():
                op("act", lambda h: h.activation(out=MS2[:], in_=MS[:], func=AF.Sqrt, bias=epsT[0:64, :], scale=1.0 / 128), reads=[MS, epsT], writes=[MS2])
                op("dve", lambda h: h.reciprocal(out=MS2[:], in_=MS2[:]), reads=[MS2], writes=[MS2])
                op("dve", lambda h: h.tensor_tensor(out=O[:], in0=O[:], in1=bc(MS2[:].unsqueeze(2), [64, NB, 128]), op=ALU.mult), reads=[O, MS2], writes=[O])
                op("pool", lambda h: h.tensor_tensor(out=O[:], in0=O[:], in1=bc(DNG[:].unsqueeze(1), [64, NB, 128]), op=ALU.mult), reads=[O, DNG], writes=[O])
                for n_ in range(NB):
                    op("pe", lambda h: h.transpose(out=bB1(128, 64 * n_, 64 * n_ + 64), in_=O[:, n_, :], identity=I64), reads=[O, identF], writes=[pbB[1]])
                op("dve", lambda h: h.tensor_tensor(out=DNT[:, hd, t0:t0 + NTK], in0=bB1(128, 0, NTK), in1=SZ[:], op=ALU.mult), reads=[pbB[1], SZ], writes=[DNT])
            th.append(q_out)
            return th
        return batch

    batches = [make_set(0), make_set(1)]
    nqb = NQB if L.get("stop_after") != "gdn1" else 1
    la, lb = [], []
    for hh in range(2):
        for qb in range(nqb):
            la += batches[0](hh, qb)
            lb += batches[1](2 + hh, qb)
    OFF = L.get("gdn_off", 0)
    ia = ib = 0
    while ia < len(la) or ib < len(lb):
        if ia < len(la):
            la[ia]()
            ia += 1
        if ia > OFF or ia >= len(la):
            if ib < len(lb):
                lb[ib]()
                ib += 1
    c.pop()


def attn_stage(c, nc, L):
    op, dma, bank, bank_bf, PB, PT = L["op"], L["dma"], L["bank"], L["bank_bf"], L["PB"], L["PT"]
    identF, identB, onesF, epsT, HT, HTv, DNT, w_in_v = (L[k] for k in ("identF", "identB", "onesF", "epsT", "HT", "HTv", "DNT", "w_in_v"))
    MODBC, GT1, rstd_from_ss = L["MODBC"], L["GT1"], L["rstd_from_ss"]
    x_d, out_d, w_out, wkv, qng, kng, kvng, ikng, relb, ohn_d = (L[k] for k in ("x_d", "out_d", "w_out", "wkv", "qng", "kng", "kvng", "ikng", "relb", "ohn_d"))
    dbg, dbg_d, stop_after = L["dbg"], L["dbg_d"], L["stop_after"]
    c.push()

    def pbs(q):
        return [PB[2 * q], PB[2 * q + 1]]

    WTM2 = c.sb("WTM2", [128, 8, 160], BF16)
    dma("pool", WTM2[:, :, 0:128], w_in_v[:, :, C_KV:C_KV + 128], writes=[WTM2])
    dma("pool", WTM2[:, :, 128:160], w_in_v[:, :, C_KIDX:C_KIDX + 32], writes=[WTM2])
    WQ = c.sb("WQ", [128, 8, 520], BF16)
    dma("pool", WQ[:, :, 0:512], w_in_v[:, :, 0:512], writes=[WQ])
    dma("pool", WQ[:, :, 512:520], w_in_v[:, :, C_WIDX:C_WIDX + 8], writes=[WQ])
    WQI = c.sb("WQI", [128, 8, 256], BF16)
    dma("pool", WQI[:], w_in_v[:, :, C_QIDX:C_QIDX + 256], writes=[WQI])
    WKV = c.sb("WKV", [128, 128], BF16)
    dma("pool", WKV[:], wkv, writes=[WKV])
    WOA = c.sb("WOA", [64, 8, 1024], BF16)
    dma("pool", WOA[:], w_out[0:512, :].rearrange("(h d) n -> d h n", d=64), writes=[WOA])
    WOD = c.sb("WOD", [128, 4, 1024], BF16)
    dma("pool", WOD[:], w_out[512:1024, :].rearrange("(q p) n -> p q n", p=128), writes=[WOD])
    op("pool", lambda h: h.tensor_tensor(out=WOA[:], in0=WOA[:], in1=bc(MODBC[0:64, 2 * D:3 * D].unsqueeze(1), [64, 8, 1024]), op=ALU.mult), reads=[WOA, MODBC], writes=[WOA])
    op("pool", lambda h: h.tensor_tensor(out=WOD[:], in0=WOD[:], in1=bc(GT1.unsqueeze(1), [128, 4, 1024]), op=ALU.mult), reads=[WOD, MODBC], writes=[WOD])
    QG = c.sb("QG", [128, 64]); dma("sp", QG[:], qng.partition_broadcast(128), writes=[QG])
    KG = c.sb("KG", [128, 64]); dma("sp", KG[:], kng.partition_broadcast(128), writes=[KG])
    KVG = c.sb("KVG", [128, 128]); dma("sp", KVG[:], kvng.partition_broadcast(128), writes=[KVG])
    IKG = c.sb("IKG", [128, 32]); dma("sp", IKG[:], ikng.partition_broadcast(128), writes=[IKG])
    RBBC = c.sb("RBBC", [128, 256]); dma("sp", RBBC[:], relb.partition_broadcast(128), writes=[RBBC])
    CN = c.sb("CN", [128, 128])
    op("pool", lambda h: h.memset(CN[:], NEG), writes=[CN])
    op("pool", lambda h: h.affine_select(out=CN[:], in_=CN[:], pattern=[[1, 128]], compare_op=ALU.is_ge, fill=0.0, base=-1,
                                         channel_multiplier=-1), reads=[CN], writes=[CN])
    BTH = [c.sb("BTH%d" % k, [128, 1024], BF16) for k in range(2)]
    BTL = [c.sb("BTL%d" % k, [128, 1024], BF16) for k in range(2)]
    JB = c.sb("JB", [128, 128], BF16)
    op("pool", lambda h: h.memset(JB[:], 1.0), writes=[JB])
    op("pool", lambda h: h.affine_select(out=JB[:], in_=JB[:], pattern=[[1, 128]], compare_op=ALU.is_equal, fill=0.0, base=-127,
                                         channel_multiplier=1), reads=[JB], writes=[JB])
    c.push()
    RB32 = c.sb("RB32", [32, 8]); OHN = c.sb("OHN", [32, 384]); VT = c.sb("VT", [8, 384])
    BTF = c.sb("BTF", [128, 8, 128])
    dma("sp", RB32[:], relb.rearrange("(b h) -> b h", h=8), writes=[RB32])
    dma("sp", OHN[:], L["ohn_d"], writes=[OHN])
    op("dve", lambda h: h.tensor_scalar(out=RB32[:], in0=RB32[:], scalar1=8.0, scalar2=None, op0=ALU.mult), reads=[RB32], writes=[RB32])
    op("pe", lambda h: h.matmul(bank(0, 8, 0, 384), lhsT=RB32[:], rhs=OHN[:], start=True, stop=True), reads=[RB32, OHN], writes=[PB[0]])
    op("act", lambda h: h.activation(out=VT[:], in_=bank(0, 8, 0, 384), func=AF.Copy), reads=[PB[0]], writes=[VT])
    scr = nc.dram_tensor("t5_scr", [8, 384], F32)
    SCR = Buf("scr", scr)
    dma("sp", scr.ap(), VT[:], reads=[VT], writes=[SCR])
    for k in range(2):
        src = bass.AP(scr, 128 * k, [[1, 128], [384, 8], [1, 128]])
        dma("sp", BTF[:], src, reads=[SCR], writes=[BTF])
        btf = BTF[:].rearrange("p h t -> p (h t)")
        op("dve", lambda h: h.tensor_copy(out=BTH[k][:], in_=btf), reads=[BTF], writes=[BTH[k]])
        op("dve", lambda h: h.tensor_tensor(out=BTL[k][:], in0=btf, in1=BTH[k][:], op=ALU.subtract), reads=[BTF, BTH[k]], writes=[BTL[k]])
    c.pop()

    KT = c.sb("KT", [65, T], BF16)
    V1 = c.sb("V1", [128, NT, 128], BF16)
    KIT = c.sb("KIT", [96, T], BF16)
    op("pool", lambda h: h.memset(KT[64:65, :], 1.0), writes=[KT])
    op("pool", lambda h: h.memset(V1[:, :, 64:128], 1.0), writes=[V1])
    QT = [c.sb("QT%d" % k, [65, 8, 128], BF16) for k in range(3)]
    for k in range(3):
        op("dve", lambda h: h.tensor_scalar(out=QT[k][64:65, :, :], in0=bc(RBBC[64:65, 248:256].unsqueeze(2), [1, 8, 128]), scalar1=8.0, scalar2=None, op0=ALU.mult),
           reads=[RBBC], writes=[QT[k]])
    c.push()
    NSL = 4
    SSa_ = [c.sb("SSa%d" % q, [128, 16]) for q in range(NSL)]
    TM2_ = [c.sb("TM2%d" % q, [128, 160]) for q in range(NSL)]; JK_ = [c.sb("JK%d" % q, [128, 128]) for q in range(NSL)]
    KVL_ = [c.sb("KVL%d" % q, [128, 128], BF16) for q in range(NSL)]; KVLT_ = [c.sb("KVLT%d" % q, [128, 128], BF16) for q in range(NSL)]
    KVs_ = [c.sb("KVs%d" % q, [128, 128]) for q in range(NSL)]
    KB_ = [c.sb("KB%d" % q, [128, 64], BF16) for q in range(NSL)]; KIB_ = [c.sb("KIB%d" % q, [128, 96], BF16) for q in range(NSL)]

    def kv_tile(i, q):
        ts = slice(128 * i, 128 * i + 128)
        SSa, TM2, JK, KVL, KVLT, KVs, KB, KIB = SSa_[q], TM2_[q], JK_[q], KVL_[q], KVLT_[q], KVs_[q], KB_[q], KIB_[q]
        b0, b1 = 2 * q, 2 * q + 1
        th = []

        def k0():
            for k in range(8):
                op("pe", lambda h: h.matmul(bank(b0, 128, 0, 160), lhsT=HT[:, k, ts], rhs=WTM2[:, k, :], start=(k == 0), stop=(k == 7)), reads=[HTv[i], WTM2], writes=[PB[b0]])
            op("act", lambda h: h.activation(out=TM2[:], in_=bank(b0, 128, 0, 160), func=AF.Copy), reads=[PB[b0]], writes=[TM2])
        th.append(k0)

        def k1():
            op("act", lambda h: h.activation(out=JK[:, 0:128], in_=TM2[:, 0:128], func=AF.Square, accum_out=SSa[:, 0:1]), reads=[TM2], writes=[JK, SSa])
            op("act", lambda h: h.activation(out=JK[:, 0:32], in_=TM2[:, 128:160], func=AF.Square, accum_out=SSa[:, 8:9]), reads=[TM2], writes=[JK, SSa])
            rstd_from_ss(SSa[:, 0:1], SSa[:, 2:3], 128, [SSa], SSa, SSa, SSa[:, 1:2])
            rstd_from_ss(SSa[:, 8:9], SSa[:, 10:11], 32, [SSa], SSa, SSa, SSa[:, 9:10])
        th.append(k1)

        def k2():
            op("dve", lambda h: h.scalar_tensor_tensor(out=KVL[:], in0=TM2[:, 0:128], scalar=SSa[:, 2:3], in1=KVG[:], op0=ALU.mult, op1=ALU.mult), reads=[TM2, SSa, KVG], writes=[KVL])
            op("dve", lambda h: h.scalar_tensor_tensor(out=KIB[:].rearrange("p (r d) -> p r d", d=32), in0=bc(TM2[:, 128:160].unsqueeze(1), [128, 3, 32]), scalar=SSa[:, 10:11],
                                                       in1=bc(IKG[:].unsqueeze(1), [128, 3, 32]), op0=ALU.mult, op1=ALU.mult), reads=[TM2, SSa, IKG], writes=[KIB])
            op("pe", lambda h: h.transpose(out=bank_bf(b1, 128, 128), in_=KVL[:], identity=identB[:]), reads=[KVL, identB], writes=[PB[b1]])
            op("pe", lambda h: h.transpose(out=bank_bf(b1, 96, 256)[:, 128:256], in_=KIB[:], identity=identB[:]), reads=[KIB, identB], writes=[PB[b1]])
            op("act", lambda h: h.activation(out=KVLT[:], in_=bank_bf(b1, 128, 128), func=AF.Copy), reads=[PB[b1]], writes=[KVLT])
            op("act", lambda h: h.activation(out=KIT[:, ts], in_=bank_bf(b1, 96, 256)[:, 128:256], func=AF.Copy), reads=[PB[b1]], writes=[KIT])
        th.append(k2)

        def k3():
            op("pe", lambda h: h.matmul(bank(b0, 128, 256, 384), lhsT=KVLT[:], rhs=WKV[:], start=True, stop=True), reads=[KVLT, WKV], writes=[PB[b0]])
            op("act", lambda h: h.activation(out=KVs[:], in_=bank(b0, 128, 256, 384), func=AF.Copy), reads=[PB[b0]], writes=[KVs])
            op("pool", lambda h: h.tensor_copy(out=V1[:, i, 0:64], in_=KVs[:, 64:128]), reads=[KVs], writes=[V1])
            op("act", lambda h: h.activation(out=JK[:, 0:64], in_=KVs[:, 0:64], func=AF.Square, accum_out=SSa[:, 4:5]), reads=[KVs], writes=[JK, SSa])
            rstd_from_ss(SSa[:, 4:5], SSa[:, 6:7], 64, [SSa], SSa, SSa, SSa[:, 5:6])
        th.append(k3)

        def k4():
            op("dve", lambda h: h.scalar_tensor_tensor(out=KB[:], in0=KVs[:, 0:64], scalar=SSa[:, 6:7], in1=KG[:], op0=ALU.mult, op1=ALU.mult), reads=[KVs, SSa, KG], writes=[KB])
            op("pe", lambda h: h.transpose(out=bank_bf(b1, 64, 384)[:, 256:384], in_=KB[:], identity=identB[:]), reads=[KB, identB], writes=[PB[b1]])
            op("act", lambda h: h.activation(out=KT[0:64, ts], in_=bank_bf(b1, 64, 384)[:, 256:384], func=AF.Copy), reads=[PB[b1]], writes=[KT])
        th.append(k4)
        return th

    for i0 in range(0, NT, NSL):
        lists = [kv_tile(i0 + q, q) for q in range(NSL)]
        for step in range(len(lists[0])):
            for q in range(NSL):
                lists[q][step]()
    c.pop()
    if "KT" in dbg:
        dbg_d["KT"] = (KT, nc.dram_tensor("dbg_KT", [65, T], BF16, kind="ExternalOutput").ap())
        dbg_d["V1"] = (V1, nc.dram_tensor("dbg_V1", [128, NT * 128], BF16, kind="ExternalOutput").ap())
        dbg_d["KIT"] = (KIT, nc.dram_tensor("dbg_KIT", [96, T], BF16, kind="ExternalOutput").ap())

    TMQ = c.sb("TMQ", [128, 520]); QQ = c.sb("QQ", [128, 512]); QNB = c.sb("QNB", [128, 512], BF16)
    MSQ = c.sb("MSQ", [128, 24]); WV = c.sb("WV", [128, 8])
    QIT = c.sb("QIT", [96, 3, 128], BF16)
    S2 = [c.sb("S%d" % k, [128, T]) for k in range(2)]; MASK = c.sb("MASK", [128, T], BF16)
    MB = [c.sb("MB%d" % k, [128, NT, 128], BF16) for k in range(2)]
    RT = [c.sb("RT%d" % k, [128, 512]) for k in range(2)]
    ST = c.sb("ST", [128, 8]); WF = c.sb("WF", [128, K_ITERS]); FROW = c.sb("FROW", [128, K_ITERS])
    EB = [c.sb("EB%d" % k, [128, 1024], BF16) for k in range(2)]
    NDs = c.sb("NDs", [128, 1024]); DEN = c.sb("DEN", [64, 1024]); ATT = c.sb("ATT", [64, 8, 128], BF16)
    XT2 = [c.sb("XT%d" % k, [128, D]) for k in range(2)]; TMP = c.sb("TMP", [128, D])
    WSC = (8.0 ** -0.5) * (32.0 ** -0.5)
    MBIG = 240000.0
    for k in range(K_ITERS):
        op("pool", lambda h: h.memset(FROW[:, k:k + 1], 2.0 * 2.0 ** -(k + 2)), writes=[FROW])
    last = NT if stop_after != "attn1" else 3

    def stageA(i, part):
        th = []
        ts = slice(128 * i, 128 * i + 128)
        Lk = 128 * (i + 1)
        qt = QT[i % 3]
        mb = MB[i % 2]
        S = S2[i % 2]

        def a0():
            for k in range(8):
                op("pe", lambda h: h.matmul(bank(6), lhsT=HT[:, k, ts], rhs=WQ[:, k, 0:512], start=(k == 0), stop=(k == 7)), reads=[HTv[i], WQ], writes=[PB[6]])
            for k in range(8):
                op("pe", lambda h: h.matmul(bank(7, 128, 0, 8), lhsT=HT[:, k, ts], rhs=WQ[:, k, 512:520], start=(k == 0), stop=(k == 7)), reads=[HTv[i], WQ], writes=[PB[7]])
            op("act", lambda h: h.activation(out=TMQ[:, 0:512], in_=bank(6), func=AF.Copy), reads=[PB[6]], writes=[TMQ])
            op("act", lambda h: h.activation(out=TMQ[:, 512:520], in_=bank(7, 128, 0, 8), func=AF.Copy), reads=[PB[7]], writes=[TMQ])
            op("pool", lambda h: h.tensor_tensor(out=QQ[:], in0=TMQ[:, 0:512], in1=TMQ[:, 0:512], op=ALU.mult), reads=[TMQ], writes=[QQ])
            op("dve", lambda h: h.tensor_reduce(out=MSQ[:, 0:8], in_=QQ[:].rearrange("p (h d) -> p h d", d=64), axis=AX.X, op=ALU.add), reads=[QQ], writes=[MSQ])
            rstd_from_ss(MSQ[:, 0:8], MSQ[:, 16:24], 64, [MSQ], MSQ, MSQ, MSQ[:, 8:16])
            op("dve", lambda h: h.tensor_tensor(out=QQ[:].rearrange("p (h d) -> p h d", d=64), in0=TMQ[:, 0:512].rearrange("p (h d) -> p h d", d=64),
                                                in1=bc(MSQ[:, 16:24].unsqueeze(2), [128, 8, 64]), op=ALU.mult), reads=[TMQ, MSQ], writes=[QQ])
            op("pool", lambda h: h.tensor_tensor(out=QNB[:].rearrange("p (h d) -> p h d", d=64), in0=QQ[:].rearrange("p (h d) -> p h d", d=64),
                                                 in1=bc(QG[:].unsqueeze(1), [128, 8, 64]), op=ALU.mult), reads=[QQ, QG], writes=[QNB])
            for hh in range(8):
                op("pe", lambda h: h.transpose(out=bank_bf(7, 64, 1024)[:, hh * 128:(hh + 1) * 128], in_=QNB[:, hh * 64:(hh + 1) * 64], identity=identB[:]),
                   reads=[QNB, identB], writes=[PB[7]])
            op("act", lambda h: h.activation(out=qt[0:64, :, :], in_=bank_bf(7, 64, 1024).rearrange("p (h t) -> p h t", t=128), func=AF.Copy), reads=[PB[7]], writes=[qt])
            op("dve", lambda h: h.tensor_scalar(out=WV[:], in0=TMQ[:, 512:520], scalar1=WSC, scalar2=None, op0=ALU.mult), reads=[TMQ], writes=[WV])
        if part == 1:
            th.append(a0)

        def a1():
            for grp in range(3):
                ncol = 96 if grp < 2 else 64
                for k in range(8):
                    op("pe", lambda h: h.matmul(bank(6, ncol, grp * 128, grp * 128 + 128), lhsT=WQI[:, k, grp * 96:grp * 96 + ncol], rhs=HT[:, k, ts],
                                                start=(k == 0), stop=(k == 7)), reads=[WQI, HTv[i]], writes=[PB[6]])
            op("act", lambda h: h.activation(out=QIT[0:96, 0:2, :], in_=bank(6, 96, 0, 256).rearrange("p (g t) -> p g t", t=128), func=AF.Copy), reads=[PB[6]], writes=[QIT])
            op("act", lambda h: h.activation(out=QIT[0:64, 2, :], in_=bank(6, 64, 256, 384), func=AF.Copy), reads=[PB[6]], writes=[QIT])
        if part == 1:
            th.append(a1)
        nch = (Lk + 511) // 512
        cnt_ = [0]
        for cc in range(nch):
            w = min(512, Lk - 512 * cc)
            for hh in range(8):
                def a2(cc=cc, w=w, hh=hh):
                    grp, r = hh // 3, hh % 3
                    bk = 6 + (cnt_[0] % 2)
                    rt = RT[cnt_[0] % 2]
                    cnt_[0] += 1
                    op("pe", lambda h: h.matmul(bank(bk, 128, 0, w), lhsT=QIT[32 * r:32 * r + 32, grp, :], rhs=KIT[32 * r:32 * r + 32, 512 * cc:512 * cc + w],
                                                start=True, stop=True), reads=[QIT, KIT], writes=[PB[bk]])
                    op("act", lambda h: h.activation(out=rt[:, 0:w], in_=bank(bk, 128, 0, w), func=AF.Relu), reads=[PB[bk]], writes=[rt])
                    if hh == 0:
                        op("dve", lambda h: h.tensor_scalar(out=S[:, 512 * cc:512 * cc + w], in0=rt[:, 0:w], scalar1=WV[:, 0:1], scalar2=None, op0=ALU.mult), reads=[rt, WV], writes=[S])
                    else:
                        op("dve", lambda h: h.scalar_tensor_tensor(out=S[:, 512 * cc:512 * cc + w], in0=rt[:, 0:w], scalar=WV[:, hh:hh + 1], in1=S[:, 512 * cc:512 * cc + w],
                                                                   op0=ALU.mult, op1=ALU.add), reads=[rt, WV, S], writes=[S])
                if part == 1:
                    th.append(a2)
        if part == 1:
            return th

        def a3():
            if i >= 2:
                op("dve", lambda h: h.tensor_reduce(out=ST[:, 0:1], in_=S[:, 0:Lk], axis=AX.X, op=ALU.max), reads=[S], writes=[ST])
                op("dve", lambda h: h.tensor_reduce(out=ST[:, 1:2], in_=S[:, 0:Lk], axis=AX.X, op=ALU.min), reads=[S], writes=[ST])
                op("dve", lambda h: h.tensor_scalar(out=ST[:, 2:3], in0=ST[:, 0:1], scalar1=ST[:, 1:2], scalar2=1.001, op0=ALU.subtract, op1=ALU.mult), reads=[ST], writes=[ST])
                op("dve", lambda h: h.tensor_tensor(out=WF[:], in0=FROW[:], in1=bc(ST[:, 2:3], [128, K_ITERS]), op=ALU.mult), reads=[FROW, ST], writes=[WF])
                op("dve", lambda h: h.scalar_tensor_tensor(out=ST[:, 4:5], in0=ST[:, 2:3], scalar=0.5, in1=ST[:, 1:2], op0=ALU.mult, op1=ALU.add), reads=[ST], writes=[ST])
            else:
                op("dve", lambda h: h.memset(ST[:, 3:4], -1.0e29), writes=[ST])
            op("dve", lambda h: h.tensor_tensor(out=S[:, ts], in0=S[:, ts], in1=CN[:], op=ALU.add), reads=[S, CN], writes=[S])
        th.append(a3)
        if i >= 2:
            for k in range(K_ITERS):
                def a4(k=k):
                    op("dve", lambda h: h.tensor_scalar(out=MASK[:, 0:Lk], in0=S[:, 0:Lk], scalar1=ST[:, 4:5], scalar2=0.0, op0=ALU.is_ge, op1=ALU.add, accum_out=ST[:, 5:6]),
                       reads=[S, ST], writes=[MASK, ST])
                    op("dve", lambda h: h.tensor_scalar(out=ST[:, 6:7], in0=ST[:, 5:6], scalar1=255.5, scalar2=0.5, op0=ALU.is_ge, op1=ALU.subtract), reads=[ST], writes=[ST])
                    if k < K_ITERS - 1:
                        op("dve", lambda h: h.scalar_tensor_tensor(out=ST[:, 4:5], in0=ST[:, 6:7], scalar=WF[:, k:k + 1], in1=ST[:, 4:5], op0=ALU.mult, op1=ALU.add), reads=[ST, WF], writes=[ST])
                    else:
                        op("dve", lambda h: h.tensor_scalar(out=ST[:, 6:7], in0=ST[:, 6:7], scalar1=0.5, scalar2=None, op0=ALU.subtract), reads=[ST], writes=[ST])
                        op("dve", lambda h: h.scalar_tensor_tensor(out=ST[:, 3:4], in0=ST[:, 6:7], scalar=WF[:, k:k + 1], in1=ST[:, 4:5], op0=ALU.mult, op1=ALU.add), reads=[ST, WF], writes=[ST])
                th.append(a4)

        def a5():
            op("dve", lambda h: h.tensor_scalar(out=MASK[:, 0:Lk], in0=S[:, 0:Lk], scalar1=ST[:, 3:4], scalar2=None, op0=ALU.is_ge), reads=[S, ST], writes=[MASK])
            if "S" in dbg and i == 2:
                dbg_d["S"] = (S, nc.dram_tensor("dbg_S", [128, T], F32, kind="ExternalOutput").ap())
                dbg_d["MASK"] = (MASK, nc.dram_tensor("dbg_MASK", [128, T], BF16, kind="ExternalOutput").ap())
            for j in range(i + 1):
                bk = 6 + j // 8
                op("pe", lambda h: h.transpose(out=bank_bf(bk)[:, (j % 8) * 128:(j % 8) * 128 + 128], in_=MASK[:, 128 * j:128 * j + 128], identity=identB[:]),
                   reads=[MASK, identB], writes=[PB[bk]])
            n6 = min(8, i + 1)
            op("act", lambda h: h.activation(out=mb[:, 0:n6, :], in_=bank_bf(6)[:, 0:n6 * 128].rearrange("p (j t) -> p j t", t=128), func=AF.Identity, scale=MBIG, bias=NEGB[:]),
               reads=[PB[6], NEGB], writes=[mb])
            if i >= 8:
                n7 = i + 1 - 8
                op("act", lambda h: h.activation(out=mb[:, 8:8 + n7, :], in_=bank_bf(7)[:, 0:n7 * 128].rearrange("p (j t) -> p j t", t=128), func=AF.Identity, scale=MBIG, bias=NEGB[:]),
                   reads=[PB[7], NEGB], writes=[mb])
        th.append(a5)
        return th

    def stageB(i):
        th = []
        ts = slice(128 * i, 128 * i + 128)
        qt = QT[i % 3]
        mb = MB[i % 2]
        qf = lambda K_, half: qt[0:K_, 4 * half:4 * half + 4, :].rearrange("p h t -> p (h t)")
        XT = XT2[i % 2]
        th.append(lambda: dma("sp", XT[:], x_d[ts, :], writes=[XT]))
        for j in range(i + 1):
            def b1(j=j):
                near = (i - j) <= 1
                K_ = 64 if near else 65
                pl = j % 2
                eb = EB[pl]
                for half in range(2):
                    o_ = PT[pl].t[:, 512 * half:512 * half + 512]
                    wr = [PB[2 * pl + half]]
                    op("pe", lambda h: h.matmul(o_, lhsT=KT[0:K_, 128 * j:128 * j + 128], rhs=qf(K_, half), start=True, stop=False), reads=[KT, qt], writes=wr)
                    op("pe", lambda h: h.matmul(o_, lhsT=identB[:], rhs=bc(mb[:, j, :].unsqueeze(1), [128, 4, 128]), start=False, stop=(not near)), reads=[identB, mb], writes=wr)
                    if near:
                        kk_ = i - j
                        op("pe", lambda h: h.matmul(o_, lhsT=JB[:], rhs=BTH[kk_][:, 512 * half:512 * half + 512], start=False, stop=False), reads=[JB, BTH[kk_]], writes=wr)
                        op("pe", lambda h: h.matmul(o_, lhsT=JB[:], rhs=BTL[kk_][:, 512 * half:512 * half + 512], start=False, stop=True), reads=[JB, BTL[kk_]], writes=wr)
                op("act", lambda h: h.activation(out=eb[:], in_=PT[pl].t[:, :], func=AF.Exp, scale=0.125), reads=pbs(pl), writes=[eb])
                for half in range(2):
                    op("pe", lambda h: h.matmul(PT[2].t[:, 512 * half:512 * half + 512], lhsT=V1[:, j, :], rhs=eb[:, 512 * half:512 * half + 512], start=(j == 0), stop=(j == i)),
                       reads=[V1, eb], writes=[PB[4 + half]])
            th.append(b1)

        def b2a():
            op("act", lambda h: h.activation(out=NDs[0:64, :], in_=PT[2].t[0:64, :], func=AF.Copy), reads=pbs(2), writes=[NDs])
            op("act", lambda h: h.activation(out=NDs[64:128, :], in_=PT[2].t[64:128, :], func=AF.Ln), reads=pbs(2), writes=[NDs])
        th.append(b2a)
        tail = []

        def b2():
            op("act", lambda h: h.activation(out=NDs[64:128, :], in_=NDs[64:128, :], func=AF.Exp, scale=-1.0), reads=[NDs], writes=[NDs])
            dma("sp", DEN[:], NDs[64:128, :], reads=[NDs], writes=[DEN])
            op("pool", lambda h: h.tensor_tensor(out=ATT[:].rearrange("p h t -> p (h t)"), in0=NDs[0:64, :], in1=DEN[:], op=ALU.mult), reads=[NDs, DEN], writes=[ATT])
            if "ATT" in dbg and i == 2:
                dbg_d["ATT"] = (ATT, nc.dram_tensor("dbg_ATT", [64, 1024], BF16, kind="ExternalOutput").ap())
        tail.append(b2)

        def b3():
            for half in range(2):
                for hh in range(8):
                    op("pe", lambda h: h.matmul(PT[0].t[:, 512 * half:512 * half + 512], lhsT=ATT[:, hh, :], rhs=WOA[:, hh, 512 * half:512 * half + 512], start=(hh == 0), stop=False),
                       reads=[ATT, WOA], writes=[PB[half]])
                for q_ in range(4):
                    op("pe", lambda h: h.matmul(PT[0].t[:, 512 * half:512 * half + 512], lhsT=DNT[:, q_, ts], rhs=WOD[:, q_, 512 * half:512 * half + 512], start=False, stop=(q_ == 3)),
                       reads=[DNT, WOD], writes=[PB[half]])
            op("act", lambda h: h.activation(out=TMP[:], in_=PT[0].t[:, :], func=AF.Copy), reads=pbs(0), writes=[TMP])
            op("pool", lambda h: h.tensor_tensor(out=XT[:], in0=TMP[:], in1=XT[:], op=ALU.add), reads=[TMP, XT], writes=[XT])
            dma("sp", out_d[ts, :], XT[:], reads=[XT])
        tail.append(b3)
        return th, tail

    NEGB = c.sb("NEGB", [128, 1])
    op("pool", lambda h: h.memset(NEGB[:], -MBIG), writes=[NEGB])
    def merge(lists):
        lists = [l for l in lists if l]
        if not lists:
            return
        n0 = len(lists[0])
        pos = [0] * len(lists)
        for ib in range(n0):
            lists[0][ib]()
            for q in range(1, len(lists)):
                tgt = ((ib + 1) * len(lists[q])) // n0
                while pos[q] < tgt:
                    lists[q][pos[q]]()
                    pos[q] += 1
        for q in range(1, len(lists)):
            while pos[q] < len(lists[q]):
                lists[q][pos[q]]()
                pos[q] += 1

    merge([stageA(0, 1)])
    merge([stageA(0, 2), stageA(1, 1) if last > 1 else []])
    prev_tail = []
    for i in range(last):
        th_b, tail_b = stageB(i)
        if prev_tail:
            blist = [th_b[0]]
            rest = th_b[1:]
            pos_ = [min(1, len(rest)), min(3, len(rest))]
            for q, f in enumerate(rest):
                if q == pos_[0]:
                    blist.append(prev_tail[0])
                if q == pos_[1]:
                    blist.append(prev_tail[1])
                blist.append(f)
            if pos_[0] >= len(rest):
                blist.append(prev_tail[0])
            if pos_[1] >= len(rest):
                blist.append(prev_tail[1])
        else:
            blist = th_b
        merge([blist, stageA(i + 1, 2) if i + 1 < last else [], stageA(i + 2, 1) if i + 2 < last else []])
        prev_tail = tail_b
    for f in prev_tail:
        f()
    c.pop()


def moe_stage(c, nc, L):
    op, dma, bank, bank_bf, PB, PT = L["op"], L["dma"], L["bank"], L["bank_bf"], L["PB"], L["PT"]
    HT, HTv, MODBC, A2, SH2, GT2, norm_to_HT = L["HT"], L["HTv"], L["MODBC"], L["A2"], L["SH2"], L["GT2"], L["norm_to_HT"]
    out_d, rw_d, rb_d, w1, w3, w2, dbg, dbg_d = (L[k] for k in ("out_d", "rw_d", "rb_d", "w1", "w3", "w2", "dbg", "dbg_d"))
    c.push()
    X = c.sb("X", [128, NT, D])
    Xv = [[c.view(X, "X%d_%d" % (i, dh)) for dh in range(2)] for i in range(NT)]
    GATES = c.sb("GATES", [128, NT, 4, 8])
    c.push()
    RW = c.sb("RW", [128, 8, 36], BF16)
    dma("pool", RW[:], rw_d.rearrange("(j p) n -> p j n", p=128), writes=[RW])
    RBB = c.sb("RBB", [128, 36]); dma("sp", RBB[:], rb_d.partition_broadcast(128), writes=[RBB])
    LG = c.sb("LG", [128, NT, 36])
    NR = 3
    SS = [c.sb("SSm%d" % k, [128, 4]) for k in range(NR)]
    TA = c.sb("TAm", [128, D]); TB = [c.sb("TBm%d" % k, [128, D]) for k in range(NR)]
    HB = [c.sb("HBm%d" % k, [128, D], BF16) for k in range(NR)]
    norm_front, norm_back = L["norm_front"], L["norm_back"]

    def m_front(i):
        ts = slice(128 * i, 128 * i + 128)
        dma("sp", X[:, i, :], out_d[ts, :], writes=Xv[i])
        norm_front(Xv[i][0], X[:, i, :], i, A2, SH2, SS[i % NR], TA, TB[i % NR], HB[i % NR], extra_reads=[Xv[i][1]])

    m_front(0)
    for i in range(NT):
        ts = slice(128 * i, 128 * i + 128)
        if i + 1 < NT:
            m_front(i + 1)
        norm_back(i, HB[i % NR], i % 2)
        bk = 2 + i % 2
        for k in range(8):
            op("pe", lambda h: h.matmul(bank(bk, 128, 0, 36), lhsT=HT[:, k, ts], rhs=RW[:, k, :], start=(k == 0), stop=(k == 7)), reads=[HTv[i], RW], writes=[PB[bk]])
        op("dve", lambda h: h.tensor_tensor(out=LG[:, i, :], in0=bank(bk, 128, 0, 36), in1=RBB[:], op=ALU.add), reads=[PB[bk], RBB], writes=[LG])
    if "LG" in dbg:
        dbg_d["LG"] = (LG, nc.dram_tensor("dbg_LG", [128, NT * 36], F32, kind="ExternalOutput").ap())
    lg = LG[:, :, 0:4]
    le = LG[:, :, 4:36].rearrange("p i (g e) -> p i g e", e=8)
    GM = c.sb("GM", [128, NT]); GOH = c.sb("GOH", [128, NT, 4]); GE = c.sb("GE", [128, NT, 4]); GS = c.sb("GS", [128, NT])
    TG = c.sb("TG", [128, NT, 4, 8]); EIN = c.sb("EIN", [128, NT, 8]); EIN2 = c.sb("EIN2", [128, NT, 8])
    M1 = c.sb("M1", [128, NT]); M2 = c.sb("M2", [128, NT]); OH1 = c.sb("OH1r", [128, NT, 8]); OH2 = c.sb("OH2r", [128, NT, 8])
    E2 = c.sb("E2r", [128, NT]); W1c = c.sb("W1c", [128, NT]); W2c = c.sb("W2c", [128, NT]); GIG = c.sb("GIG", [128, NT, 8])
    dv = lambda fn, reads, writes: op("dve", fn, reads=reads, writes=writes)
    dv(lambda h: h.tensor_reduce(out=GM[:], in_=lg, axis=AX.X, op=ALU.max), [LG], [GM])
    dv(lambda h: h.tensor_tensor(out=GOH[:], in0=lg, in1=bc(GM[:].unsqueeze(2), [128, NT, 4]), op=ALU.is_ge), [LG, GM], [GOH])
    dv(lambda h: h.tensor_tensor(out=GE[:], in0=lg, in1=bc(GM[:].unsqueeze(2), [128, NT, 4]), op=ALU.subtract), [LG, GM], [GE])
    op("act", lambda h: h.activation(out=GE[:], in_=GE[:], func=AF.Exp), reads=[GE], writes=[GE])
    dv(lambda h: h.tensor_reduce(out=GS[:], in_=GE[:], axis=AX.X, op=ALU.add), [GE], [GS])
    dv(lambda h: h.reciprocal(out=GS[:], in_=GS[:]), [GS], [GS])
    dv(lambda h: h.tensor_tensor(out=TG[:], in0=le, in1=bc(GOH[:].unsqueeze(3), [128, NT, 4, 8]), op=ALU.mult), [LG, GOH], [TG])
    dv(lambda h: h.tensor_reduce(out=EIN[:], in_=TG[:].rearrange("p i g e -> p i e g"), axis=AX.X, op=ALU.add), [TG], [EIN])
    dv(lambda h: h.tensor_reduce(out=M1[:], in_=EIN[:], axis=AX.X, op=ALU.max), [EIN], [M1])
    dv(lambda h: h.tensor_tensor(out=OH1[:], in0=EIN[:], in1=bc(M1[:].unsqueeze(2), [128, NT, 8]), op=ALU.is_ge), [EIN, M1], [OH1])
    dv(lambda h: h.scalar_tensor_tensor(out=EIN2[:], in0=OH1[:], scalar=-1.0e30, in1=EIN[:], op0=ALU.mult, op1=ALU.add), [OH1, EIN], [EIN2])
    dv(lambda h: h.tensor_reduce(out=M2[:], in_=EIN2[:], axis=AX.X, op=ALU.max), [EIN2], [M2])
    dv(lambda h: h.tensor_tensor(out=OH2[:], in0=EIN2[:], in1=bc(M2[:].unsqueeze(2), [128, NT, 8]), op=ALU.is_ge), [EIN2, M2], [OH2])
    dv(lambda h: h.tensor_tensor(out=E2[:], in0=M2[:], in1=M1[:], op=ALU.subtract), [M1, M2], [E2])
    op("act", lambda h: h.activation(out=E2[:], in_=E2[:], func=AF.Exp), reads=[E2], writes=[E2])
    dv(lambda h: h.tensor_scalar(out=W1c[:], in0=E2[:], scalar1=1.0, scalar2=None, op0=ALU.add), [E2], [W1c])
    dv(lambda h: h.reciprocal(out=W1c[:], in_=W1c[:]), [W1c], [W1c])
    dv(lambda h: h.tensor_tensor(out=W1c[:], in0=W1c[:], in1=GS[:], op=ALU.mult), [W1c, GS], [W1c])
    dv(lambda h: h.tensor_tensor(out=W2c[:], in0=W1c[:], in1=E2[:], op=ALU.mult), [W1c, E2], [W2c])
    dv(lambda h: h.tensor_tensor(out=OH1[:], in0=OH1[:], in1=bc(W1c[:].unsqueeze(2), [128, NT, 8]), op=ALU.mult), [OH1, W1c], [OH1])
    dv(lambda h: h.tensor_tensor(out=OH2[:], in0=OH2[:], in1=bc(W2c[:].unsqueeze(2), [128, NT, 8]), op=ALU.mult), [OH2, W2c], [OH2])
    dv(lambda h: h.tensor_tensor(out=GIG[:], in0=OH1[:], in1=OH2[:], op=ALU.add), [OH1, OH2], [GIG])
    dv(lambda h: h.tensor_tensor(out=GATES[:], in0=bc(GOH[:].unsqueeze(3), [128, NT, 4, 8]), in1=bc(GIG[:].unsqueeze(2), [128, NT, 4, 8]), op=ALU.mult), [GOH, GIG], [GATES])
    if "GATES" in dbg:
        dbg_d["GATES"] = (GATES, nc.dram_tensor("dbg_GATES", [128, NT * 32], F32, kind="ExternalOutput").ap())
    c.pop()
    GAT = GATES[:].rearrange("p i g e -> p i (g e)")

    W1B = [c.sb("W1B%d" % k, [128, 8, 512], BF16) for k in range(2)]
    W3B = [c.sb("W3B%d" % k, [128, 8, 512], BF16) for k in range(2)]
    W2B = [c.sb("W2B%d" % k, [128, 4, 1024], BF16) for k in range(2)]
    SL = [c.sb("SL%d" % k, [128, 512]) for k in range(2)]
    ACTT = [c.sb("ACTT%d" % k, [128, 4, 512], BF16) for k in range(2)]
    n_exp = L.get("n_exp", 32)
    for e in range(n_exp):
        wb = e % 2
        dma("pool", W1B[wb][:], w1[e].rearrange("(j p) n -> p j n", p=128), writes=[W1B[wb]])
        dma("pool", W3B[wb][:], w3[e].rearrange("(j p) n -> p j n", p=128), writes=[W3B[wb]])
        dma("pool", W2B[wb][:], w2[e].rearrange("(j p) n -> p j n", p=128), writes=[W2B[wb]])
        op("pool", lambda h: h.tensor_tensor(out=W2B[wb][:], in0=W2B[wb][:], in1=bc(GT2.unsqueeze(1), [128, 4, 1024]), op=ALU.mult), reads=[W2B[wb], MODBC], writes=[W2B[wb]])
        for tb in range(4):
            at = ACTT[tb % 2]
            tiles = [HTv[4 * tb + q] for q in range(4)]
            for fc in range(4):
                bkA = (fc % 2) * 2
                bkB = bkA + 1
                sl_ = SL[fc % 2]
                for k in range(8):
                    op("pe", lambda h: h.matmul(bank(bkA), lhsT=W1B[wb][:, k, 128 * fc:128 * fc + 128], rhs=HT[:, k, 512 * tb:512 * tb + 512], start=(k == 0), stop=(k == 7)),
                       reads=[W1B[wb]] + tiles, writes=[PB[bkA]])
                for k in range(8):
                    op("pe", lambda h: h.matmul(bank(bkB), lhsT=W3B[wb][:, k, 128 * fc:128 * fc + 128], rhs=HT[:, k, 512 * tb:512 * tb + 512], start=(k == 0), stop=(k == 7)),
                       reads=[W3B[wb]] + tiles, writes=[PB[bkB]])
                op("act", lambda h: h.activation(out=sl_[:], in_=bank(bkA), func=AF.Silu), reads=[PB[bkA]], writes=[sl_])
                op("dve", lambda h: h.tensor_tensor(out=at[:, fc, :], in0=sl_[:], in1=bank(bkB), op=ALU.mult), reads=[sl_, PB[bkB]], writes=[at])
            for tt in range(4):
                tile = 4 * tb + tt
                for dh in range(2):
                    bkY = 4 + (2 * tt + dh) % 4
                    for fc in range(4):
                        op("pe", lambda h: h.matmul(bank(bkY), lhsT=at[:, fc, 128 * tt:128 * tt + 128], rhs=W2B[wb][:, fc, 512 * dh:512 * dh + 512], start=(fc == 0), stop=(fc == 3)),
                           reads=[at, W2B[wb]], writes=[PB[bkY]])
                    op("dve", lambda h: h.scalar_tensor_tensor(out=X[:, tile, 512 * dh:512 * dh + 512], in0=bank(bkY), scalar=GAT[:, tile, e:e + 1], in1=X[:, tile, 512 * dh:512 * dh + 512],
                                                               op0=ALU.mult, op1=ALU.add), reads=[PB[bkY], GATES, Xv[tile][dh]], writes=[Xv[tile][dh]])
    for i in range(NT):
        dma("sp", out_d[128 * i:128 * i + 128, :], X[:, i, :], reads=Xv[i])
    c.pop()


def make_inputs(inp, b):
    f = lambda a: np.ascontiguousarray(np.asarray(a, dtype=np.float32))
    m = {}
    m["x"] = f(inp["x"][b])
    m["c_fm"] = f(np.asarray(inp["c"][b]).reshape(8, 128).T)
    m["ada_w"] = f(inp["ada_w"][0])
    m["ada_b"] = f(inp["ada_b"][0])
    m["norm1_g"] = f(inp["norm1_g"][0])
    m["norm2_g"] = f(inp["norm2_g"][0])
    m["w_in"] = f(inp["w_in"][0])
    m["q_norm_g"] = f(inp["q_norm_g"][0])
    m["k_norm_g"] = f(inp["k_norm_g"][0])
    m["kv_norm_g"] = f(inp["kv_norm_g"][0])
    m["w_kv_up"] = f(inp["w_kv_up"][0])
    m["idx_k_norm_g"] = f(inp["idx_k_norm_g"][0])
    m["rel_bias"] = f(np.asarray(inp["rel_bias"]).reshape(256))
    m["conv_w_fm"] = f(np.asarray(inp["conv_w"][0]).reshape(4, 12, 128).transpose(2, 1, 0))
    m["a_log"] = f(inp["a_log"][0])
    m["dt_bias"] = f(inp["dt_bias"][0])
    m["dn_norm_g"] = f(inp["dn_norm_g"][0])
    m["w_out"] = f(inp["w_out"][0])
    m["router_w"] = f(np.concatenate([np.asarray(inp["router_g_w"][0]), np.asarray(inp["router_e_w"][0])], axis=1))
    m["router_b"] = f(np.concatenate([np.asarray(inp["router_g_b"][0]), np.asarray(inp["router_e_b"][0])], axis=0))
    m["w1"] = f(inp["w1"][0])
    m["w3"] = f(inp["w3"][0])
    m["w2"] = f(inp["w2"][0])
    m["bk_ohn"] = bucket_onehot()
    return m


def kernel(**inputs):
    nc = build()
    shared = None
    in_maps = []
    for b in range(8):
        m = make_inputs(inputs, b) if shared is None else dict(shared)
        if shared is None:
            shared = m
        else:
            m["x"] = np.ascontiguousarray(np.asarray(inputs["x"][b], dtype=np.float32))
            m["c_fm"] = np.ascontiguousarray(np.asarray(inputs["c"][b], dtype=np.float32).reshape(8, 128).T)
        in_maps.append(m)
    res = run_bass_kernel_spmd(nc, in_maps, core_ids=list(range(8)))
    return np.stack([np.asarray(r["out"], dtype=np.float32) for r in res.results], axis=0)
```
